# Optimizing a Trainium2 kernel written in Bass

```python
import math
import jax, jax.numpy as jnp
from jax import lax
import numpy as np

D_MODEL = 2048
BATCH = 4
SEQ = 4096
DEPTH = 2

GRID_W = 64
CTX_LEN = 256
N_MOD = 6
NORM_EPS = 1e-6

A_DIM = 3 * D_MODEL // 8
A_HEAD_DIM = 64
A_HEADS = A_DIM // A_HEAD_DIM
A_DECAY_RANK = 64
A_ICLR_RANK = 64
A_GATE_RANK = 128
A_DECAY_SCALE = math.exp(-0.5)
A_GN_EPS = 64e-5

B_DIM = 3 * D_MODEL // 8
B_HEADS = 4
B_KEY_DIM = B_DIM // 2
B_HEAD_K = B_KEY_DIM // B_HEADS
B_HEAD_V = B_DIM // B_HEADS
B_GATE_RANK = 16
GLA_TAU = 16.0
GLA_CHUNK = 64

C_DIM = D_MODEL // 4
C_GROUP = 16
C_GROUPS = C_DIM // C_GROUP
C_STATE = 64
S5_DT_MIN = 1e-3
S5_DT_MAX = 1e-1

N_BRANCH = 3
CONV_DIM = 3 * A_DIM + 2 * B_KEY_DIM + B_DIM
IN_SPLITS = (A_DIM, A_DIM, A_DIM, B_KEY_DIM, B_KEY_DIM, B_DIM,
             2 * A_DECAY_RANK, 2 * A_ICLR_RANK, A_GATE_RANK, 2 * B_GATE_RANK,
             B_DIM, C_DIM, N_BRANCH * D_MODEL)
IN_DIM = sum(IN_SPLITS)
IN_OFFSETS = tuple(int(o) for o in np.cumsum(IN_SPLITS)[:-1])

N_EXPERTS = 16
N_EXPERT_GROUPS = 4
EXPERTS_PER_GROUP = N_EXPERTS // N_EXPERT_GROUPS
TOP_K = 2
D_EXPERT = D_MODEL // 2

kernel_name = "hybrid_rwkv7_gla_s5_moe_prefix_dit"


def rmsnorm(x, g):
    xf = x.astype(jnp.float32)
    y = xf * lax.rsqrt(jnp.mean(xf * xf, axis=-1, keepdims=True) + NORM_EPS)
    return y.astype(x.dtype) * g


def grid_dwconv(z, w, rows, cols):
    nb, n, ch = z.shape
    out = lax.conv_general_dilated(z.reshape(nb, rows, cols, ch), w[:, :, None, :], (1, 1), "SAME",
                                   dimension_numbers=("NHWC", "HWIO", "NHWC"), feature_group_count=ch)
    return out.reshape(nb, n, ch)


def orient(z):
    return jnp.stack([z[0], jnp.flip(z[1], axis=1)])


def both(z):
    return jnp.stack([z, jnp.flip(z, axis=1)])


def in_features(h, w_in, conv_w, rows, cols):
    p = h @ w_in
    p = jnp.concatenate([grid_dwconv(p[..., :CONV_DIM], conv_w, rows, cols), p[..., CONV_DIM:]], axis=-1)
    return jnp.split(p, IN_OFFSETS, axis=-1)


def rwkv7_branch(r, k, v, d_w, d_a, d_g, S0, w0, w2, a0, a2, g2, k_k, k_a, r_k, gn_w, gn_b, need_out):
    f32 = jnp.float32
    hd = lambda z: z.reshape(z.shape[:-1] + (A_HEADS, A_HEAD_DIM))
    tm = lambda z: jnp.moveaxis(z.astype(f32), 2, 0)
    w = jnp.exp(-A_DECAY_SCALE * jax.nn.sigmoid(
        (w0[:, None, None, :] + jnp.einsum("btdr,drc->dbtc", jnp.tanh(d_w), w2)).astype(f32)))
    a = jax.nn.sigmoid((a0[:, None, None, :] + jnp.einsum("btdr,drc->dbtc", d_a, a2)).astype(f32))
    kk = hd(k * k_k).astype(f32)
    kk = kk * lax.rsqrt(jnp.sum(kk * kk, axis=-1, keepdims=True) + 1e-12)
    kd = k.astype(f32)[None] * (1.0 + (a - 1.0) * k_a.astype(f32))
    xs = (tm(both(hd(r))), tm(orient(hd(w))), tm(orient(hd(kd))), tm(both(hd(v))),
          tm(both(kk)), tm(orient(hd(a))))

    def step(S, inp):
        r_t, w_t, k_t, v_t, kk_t, a_t = inp
        sa = jnp.einsum("dbhvk,dbhk->dbhv", S, kk_t)
        S = (S * w_t[..., None, :] - jnp.einsum("dbhv,dbhk->dbhvk", sa, kk_t * a_t)
             + jnp.einsum("dbhv,dbhk->dbhvk", v_t, k_t))
        return S, (jnp.einsum("dbhvk,dbhk->dbhv", S, r_t) if need_out else None)

    S_fin, y = lax.scan(step, S0, xs)
    if not need_out:
        return None, S_fin
    y = orient(jnp.moveaxis(y, 0, 2))
    mu = jnp.mean(y, axis=-1, keepdims=True)
    var = jnp.mean(jnp.square(y - mu), axis=-1, keepdims=True)
    y = (y - mu) * lax.rsqrt(var + A_GN_EPS) * hd(gn_w) + hd(gn_b)
    rf, vf = hd(r).astype(f32), hd(v).astype(f32)
    bonus = jnp.sum(rf[None] * hd(kd) * hd(r_k), axis=-1, keepdims=True) * vf[None]
    y = jnp.sum(y + bonus, axis=0).reshape(r.shape).astype(r.dtype)
    return y * (jax.nn.sigmoid(d_g) @ g2), S_fin


def gla_chunked(q, k, v, g, S0, need_out):
    nd, nb, t, nh, _ = q.shape
    nc = t // GLA_CHUNK
    blk = lambda z: z.reshape(nd, nb, nc, GLA_CHUNK, nh, z.shape[-1])
    q, k, v, g = blk(q), blk(k), blk(v), blk(g)
    cg = jnp.cumsum(g, axis=3)
    tot = cg[:, :, :, -1]
    U = jnp.einsum("dbnihk,dbnihv->dbnhkv", k * jnp.exp(tot[:, :, :, None] - cg), v)

    def step(S, inp):
        log_a, u_n = inp
        return jnp.exp(log_a)[..., None] * S + u_n, S

    S_fin, S_start = lax.scan(step, S0, (jnp.moveaxis(tot, 2, 0), jnp.moveaxis(U, 2, 0)))
    if not need_out:
        return None, S_fin
    S_start = jnp.moveaxis(S_start, 0, 2)
    inter = jnp.einsum("dbnihk,dbnhkv->dbnihv", q * jnp.exp(cg), S_start)
    ref = cg[:, :, :, GLA_CHUNK // 2 - 1:GLA_CHUNK // 2]
    scores = jnp.einsum("dbnihk,dbnjhk->dbnhij", q * jnp.exp(cg - ref), k * jnp.exp(ref - cg))
    mask = jnp.tril(jnp.ones((GLA_CHUNK, GLA_CHUNK), dtype=bool))
    intra = jnp.einsum("dbnhij,dbnjhv->dbnihv", jnp.where(mask, scores, 0.0), v)
    return (inter + intra).reshape(nd, nb, t, nh, v.shape[-1]), S_fin


def gla_branch(q, k, v, og, d_a, S0, a2, ab, norm_g, need_out):
    f32 = jnp.float32
    hk = lambda z: z.reshape(z.shape[:-1] + (B_HEADS, B_HEAD_K))
    hv = lambda z: z.reshape(z.shape[:-1] + (B_HEADS, B_HEAD_V))
    g = jax.nn.log_sigmoid((jnp.einsum("btdr,drc->dbtc", d_a, a2) + ab[:, None, None, :]).astype(f32)) / GLA_TAU
    qh = both(hk(q).astype(f32) * (B_HEAD_K ** -0.5))
    kh = both(hk(k).astype(f32))
    vh = both(hv(v).astype(f32))
    o, S_fin = gla_chunked(qh, kh, vh, orient(hk(g)), S0, need_out)
    if not need_out:
        return None, S_fin
    o = rmsnorm(jnp.sum(orient(o), axis=0), norm_g)
    return o.reshape(v.shape).astype(v.dtype) * jax.nn.silu(og), S_fin


def s5_discretise(lam_re, lam_im, log_dt, b_re, b_im):
    f32 = jnp.float32
    lr, li = lam_re.astype(f32), lam_im.astype(f32)
    dt = jnp.exp(log_dt.astype(f32))[..., None]
    mag = jnp.exp(lr * dt)
    ar, ai = mag * jnp.cos(li * dt), mag * jnp.sin(li * dt)
    den = lr * lr + li * li
    fr = ((ar - 1.0) * lr + ai * li) / den
    fi = (ai * lr - (ar - 1.0) * li) / den
    br, bi = b_re.astype(f32), b_im.astype(f32)
    bbr = fr[..., None] * br - fi[..., None] * bi
    bbi = fr[..., None] * bi + fi[..., None] * br
    return ar, ai, bbr, bbi


def s5_combine(e1, e2):
    a1r, a1i, b1r, b1i = e1
    a2r, a2i, b2r, b2i = e2
    return (a1r * a2r - a1i * a2i, a1r * a2i + a1i * a2r,
            a2r * b1r - a2i * b1i + b2r, a2r * b1i + a2i * b1r + b2i)


def s5_branch(u, h0r, h0i, lam_re, lam_im, log_dt, b_re, b_im, c_re, c_im, d_skip, glu_w, need_out):
    f32 = jnp.float32
    ug = u.astype(f32).reshape(u.shape[:-1] + (C_GROUPS, C_GROUP))
    ar, ai, bbr, bbi = s5_discretise(lam_re, lam_im, log_dt, b_re, b_im)
    bur = orient(jnp.einsum("dgpc,btgc->dbtgp", bbr, ug))
    bui = orient(jnp.einsum("dgpc,btgc->dbtgp", bbi, ug))
    bur = bur.at[:, :, 0].add(ar[:, None] * h0r - ai[:, None] * h0i)
    bui = bui.at[:, :, 0].add(ar[:, None] * h0i + ai[:, None] * h0r)
    shp = bur.shape
    _, _, xr, xi = lax.associative_scan(
        s5_combine, (jnp.broadcast_to(ar[:, None, None], shp), jnp.broadcast_to(ai[:, None, None], shp), bur, bui),
        axis=2)
    hr, hi = xr[:, :, -1], xi[:, :, -1]
    if not need_out:
        return None, hr, hi
    xr, xi = jnp.sum(orient(xr), axis=0), jnp.sum(orient(xi), axis=0)
    y = jnp.einsum("gcp,btgp->btgc", c_re, xr) - jnp.einsum("gcp,btgp->btgc", c_im, xi)
    y = jax.nn.gelu(y.reshape(u.shape).astype(u.dtype) + d_skip * u)
    ya, yg = jnp.split(y @ glu_w, 2, axis=-1)
    return ya * jax.nn.sigmoid(yg), hr, hi


def merge(ya, yb, yc, gates, pa, pb, pc, wo):
    ga, gb, gc = jnp.split(jax.nn.sigmoid(gates), N_BRANCH, axis=-1)
    return (ga * (ya @ pa) + gb * (yb @ pb) + gc * (yc @ pc)) @ wo


def moe_ffn(h, router_w, router_bias, w1, w3, w2):
    f32 = jnp.float32
    scores = jax.nn.sigmoid((h @ router_w).astype(f32))
    biased = scores + router_bias.astype(f32)
    grp = biased.reshape(biased.shape[:-1] + (N_EXPERT_GROUPS, EXPERTS_PER_GROUP))
    gsel = jnp.argmax(jnp.sum(lax.top_k(grp, TOP_K)[0], axis=-1), axis=-1)
    in_grp = (jnp.arange(N_EXPERT_GROUPS) == gsel[..., None])[..., None]
    masked = jnp.where(in_grp, grp, -jnp.inf).reshape(biased.shape)
    _, idx = lax.top_k(masked, TOP_K)
    wsel = jnp.take_along_axis(scores, idx, axis=-1)
    wsel = wsel / jnp.sum(wsel, axis=-1, keepdims=True)
    gate = jnp.sum(jax.nn.one_hot(idx, N_EXPERTS, dtype=f32) * wsel[..., None], axis=-2).astype(h.dtype)
    out = jnp.zeros_like(h)
    for e in range(N_EXPERTS):
        he = jax.nn.silu(h @ w1[e]) * (h @ w3[e])
        out = out + gate[..., e:e + 1] * (he @ w2[e])
    return out


def setup_inputs(seed: int = 0) -> dict:
    key = jax.random.key(seed)
    ks = iter(jax.random.split(key, 64))

    def nrm(shape, scale):
        return scale * jax.random.normal(next(ks), shape, jnp.float32)

    L, D = DEPTH, D_MODEL
    G, P = C_GROUPS, C_STATE
    centre = jnp.zeros((3, 3, 1), jnp.float32).at[1, 1, 0].set(1.0)
    lam_im0 = jnp.pi * jnp.arange(P, dtype=jnp.float32)
    return {
        "x": nrm((BATCH, SEQ, D), 1.0),
        "c": nrm((BATCH, D), 1.0),
        "ctx": nrm((BATCH, CTX_LEN, D), 1.0),
        "c_ctx": nrm((D,), 1.0),
        "mod_w": nrm((L, D, N_MOD * D), 0.5 * D ** -0.5),
        "mod_b": nrm((L, N_MOD * D), 0.02),
        "norm1_g": 1.0 + nrm((L, D), 0.1),
        "norm2_g": 1.0 + nrm((L, D), 0.1),
        "w_in": nrm((L, D, IN_DIM), D ** -0.5),
        "conv_w": centre + nrm((L, 3, 3, CONV_DIM), 0.2),
        "rk_w0": nrm((L, 2, A_DIM), 0.5),
        "rk_w2": nrm((L, 2, A_DECAY_RANK, A_DIM), A_DECAY_RANK ** -0.5),
        "rk_a0": nrm((L, 2, A_DIM), 0.1),
        "rk_a2": nrm((L, 2, A_ICLR_RANK, A_DIM), A_ICLR_RANK ** -0.5),
        "rk_g2": nrm((L, A_GATE_RANK, A_DIM), A_GATE_RANK ** -0.5),
        "rk_kk": 0.85 + nrm((L, A_DIM), 0.1),
        "rk_ka": 1.0 + nrm((L, A_DIM), 0.1),
        "rk_rk": nrm((L, A_DIM), 0.1),
        "rk_gn_w": 1.0 + nrm((L, A_DIM), 0.1),
        "rk_gn_b": nrm((L, A_DIM), 0.02),
        "gla_a2": nrm((L, 2, B_GATE_RANK, B_KEY_DIM), B_GATE_RANK ** -0.5),
        "gla_ab": nrm((L, 2, B_KEY_DIM), 0.1),
        "gla_norm_g": 1.0 + nrm((L, B_HEAD_V), 0.1),
        "s5_lam_re": -0.5 + nrm((L, 2, G, P), 0.01),
        "s5_lam_im": lam_im0 + nrm((L, 2, G, P), 0.01),
        "s5_log_dt": jax.random.uniform(next(ks), (L, 2, G), jnp.float32, math.log(S5_DT_MIN), math.log(S5_DT_MAX)),
        "s5_b_re": nrm((L, G, P, C_GROUP), (2.0 * C_GROUP) ** -0.5),
        "s5_b_im": nrm((L, G, P, C_GROUP), (2.0 * C_GROUP) ** -0.5),
        "s5_c_re": nrm((L, G, C_GROUP, P), (2.0 * P) ** -0.5),
        "s5_c_im": nrm((L, G, C_GROUP, P), (2.0 * P) ** -0.5),
        "s5_d": nrm((L, C_DIM), 0.5),
        "s5_glu_w": nrm((L, C_DIM, 2 * C_DIM), C_DIM ** -0.5),
        "proj_a": nrm((L, A_DIM, D), A_DIM ** -0.5),
        "proj_b": nrm((L, B_DIM, D), B_DIM ** -0.5),
        "proj_c": nrm((L, C_DIM, D), C_DIM ** -0.5),
        "w_out": nrm((L, D, D), D ** -0.5),
        "router_w": nrm((D, N_EXPERTS), D ** -0.5),
        "router_bias": nrm((N_EXPERTS,), 0.01),
        "exp_w1": nrm((L, N_EXPERTS, D, D_EXPERT), D ** -0.5),
        "exp_w3": nrm((L, N_EXPERTS, D, D_EXPERT), D ** -0.5),
        "exp_w2": nrm((L, N_EXPERTS, D_EXPERT, D), D_EXPERT ** -0.5),
        "final_g": 1.0 + nrm((D,), 0.1),
    }


def reference(x, c, ctx, c_ctx, mod_w, mod_b, norm1_g, norm2_g, w_in, conv_w,
              rk_w0, rk_w2, rk_a0, rk_a2, rk_g2, rk_kk, rk_ka, rk_rk, rk_gn_w, rk_gn_b,
              gla_a2, gla_ab, gla_norm_g,
              s5_lam_re, s5_lam_im, s5_log_dt, s5_b_re, s5_b_im, s5_c_re, s5_c_im, s5_d, s5_glu_w,
              proj_a, proj_b, proj_c, w_out, router_w, router_bias, exp_w1, exp_w3, exp_w2, final_g):
    f32 = jnp.float32
    nb, t_lat, _ = x.shape
    t_ctx = ctx.shape[1]
    rows = t_lat // GRID_W
    pair = lambda z: z.reshape(z.shape[:-1] + (2, z.shape[-1] // 2))
    for l in range(DEPTH):
        last = l == DEPTH - 1
        sh1, sc1, ga1, sh2, sc2, ga2 = jnp.split((jax.nn.silu(c) @ mod_w[l] + mod_b[l])[:, None, :], N_MOD, axis=-1)
        csh1, csc1, cga1, csh2, csc2, cga2 = jnp.split(jax.nn.silu(c_ctx) @ mod_w[l] + mod_b[l], N_MOD, axis=-1)
        hl = rmsnorm(x, norm1_g[l]) * (1.0 + sc1) + sh1
        hc = rmsnorm(ctx, norm1_g[l]) * (1.0 + csc1) + csh1
        fc = in_features(hc, w_in[l], conv_w[l], 1, t_ctx)
        fl = in_features(hl, w_in[l], conv_w[l], rows, GRID_W)

        def mix_a(f, S0, need):
            return rwkv7_branch(f[0], f[1], f[2], pair(f[6]), pair(f[7]), f[8], S0,
                                rk_w0[l], rk_w2[l], rk_a0[l], rk_a2[l], rk_g2[l], rk_kk[l], rk_ka[l],
                                rk_rk[l], rk_gn_w[l], rk_gn_b[l], need)

        def mix_b(f, S0, need):
            return gla_branch(f[3], f[4], f[5], f[10], pair(f[9]), S0, gla_a2[l], gla_ab[l], gla_norm_g[l], need)

        def mix_c(f, h0r, h0i, need):
            return s5_branch(f[11], h0r, h0i, s5_lam_re[l], s5_lam_im[l], s5_log_dt[l], s5_b_re[l], s5_b_im[l],
                             s5_c_re[l], s5_c_im[l], s5_d[l], s5_glu_w[l], need)

        ya_c, st_a = mix_a(fc, jnp.zeros((2, nb, A_HEADS, A_HEAD_DIM, A_HEAD_DIM), f32), not last)
        yb_c, st_b = mix_b(fc, jnp.zeros((2, nb, B_HEADS, B_HEAD_K, B_HEAD_V), f32), not last)
        h0 = jnp.zeros((2, nb, C_GROUPS, C_STATE), f32)
        yc_c, st_cr, st_ci = mix_c(fc, h0, h0, not last)
        ya_l, _ = mix_a(fl, st_a, True)
        yb_l, _ = mix_b(fl, st_b, True)
        yc_l, _, _ = mix_c(fl, st_cr, st_ci, True)
        x = x + ga1 * merge(ya_l, yb_l, yc_l, fl[12], proj_a[l], proj_b[l], proj_c[l], w_out[l])
        hl2 = rmsnorm(x, norm2_g[l]) * (1.0 + sc2) + sh2
        if last:
            x = x + ga2 * moe_ffn(hl2, router_w, router_bias, exp_w1[l], exp_w3[l], exp_w2[l])
        else:
            ctx = ctx + cga1 * merge(ya_c, yb_c, yc_c, fc[12], proj_a[l], proj_b[l], proj_c[l], w_out[l])
            hc2 = rmsnorm(ctx, norm2_g[l]) * (1.0 + csc2) + csh2
            y2 = moe_ffn(jnp.concatenate([hc2, hl2], axis=1), router_w, router_bias, exp_w1[l], exp_w3[l], exp_w2[l])
            ctx = ctx + cga2 * y2[:, :t_ctx]
            x = x + ga2 * y2[:, t_ctx:]
    return rmsnorm(x, final_g)
```

```python
from concourse.bass_utils import run_bass_kernel_spmd
import numpy as np
from contextlib import ExitStack
import concourse.bass as bass
import concourse.mybir as mybir

F32 = mybir.dt.float32
BF16 = mybir.dt.bfloat16
ALU = mybir.AluOpType
AF = mybir.ActivationFunctionType
AX = mybir.AxisListType


class KB:
    def __init__(self, nc, n_dma_sems=40):
        self.nc = nc
        self.es = ExitStack()
        self.engs = {'pe': nc.tensor, 'dve': nc.vector, 'act': nc.scalar, 'pool': nc.gpsimd, 'sp': nc.sync}
        self.sems = {}
        self.cnt = {}
        for e in self.engs:
            self.sems[e] = self.es.enter_context(nc.semaphore('s_' + e))
            self.cnt[e] = 0
        self.ndma = n_dma_sems
        for i in range(n_dma_sems):
            k = 'd%d' % i
            self.sems[k] = self.es.enter_context(nc.semaphore('s_' + k))
            self.cnt[k] = 0
        self.dma_rr = 0
        self.known = {e: {} for e in self.engs}
        self.acc = {}
        self.rows = {}
        self.ninst = 0
        self.psum_names = set()

    def sbuf(self, name, shape, dt=F32):
        t = self.es.enter_context(self.nc.sbuf_tensor(name, list(shape), dt))
        self.rows[name] = int(np.prod(shape[1:]))
        return t

    def psum(self, name, shape, dt=F32):
        t = self.es.enter_context(self.nc.psum_tensor(name, list(shape), dt))
        self.rows[name] = int(np.prod(shape[1:]))
        self.psum_names.add(name)
        return t

    def dram(self, name, shape, dt=F32, kind="Internal"):
        t = self.nc.dram_tensor(name, list(shape), dt, kind=kind)
        return t

    def track_dram(self, name):
        self.rows[name] = None

    def _bbox(self, ap):
        name = ap.tensor.name
        if name not in self.rows:
            return None
        row = self.rows[name]
        if row is None or name in self.psum_names:
            return (name, 0, 1 << 30, 0, 1 << 30)
        a = ap.ap
        off = ap.offset
        p0 = off // row
        f0 = off % row
        if a[0][0] == row:
            p1 = p0 + a[0][1]
            rest = a[1:]
        elif a[0][0] == 0 and len(a) > 1:
            p1 = p0 + 1
            rest = a[1:]
        else:
            p1 = p0 + 1
            rest = a
        ext = 0
        for st, c in rest:
            ext += abs(st) * (c - 1)
        return (name, p0, p1, f0, f0 + ext + 1)

    def _deps(self, e, outs, ins):
        deps = {}

        def need(sk, v):
            if deps.get(sk, 0) < v:
                deps[sk] = v

        boxes_in = [b for b in (self._bbox(a) for a in ins) if b is not None]
        boxes_out = [b for b in (self._bbox(a) for a in outs) if b is not None]
        for (name, p0, p1, f0, f1) in boxes_in:
            ps_ = name in self.psum_names
            for ent in self.acc.get(name, ()):
                if (ent[4] or (ps_ and ent[5] != e)) and ent[0] < p1 and p0 < ent[1] and ent[2] < f1 and f0 < ent[3]:
                    need(ent[5], ent[6])
        for (name, p0, p1, f0, f1) in boxes_out:
            for ent in self.acc.get(name, ()):
                if ent[0] < p1 and p0 < ent[1] and ent[2] < f1 and f0 < ent[3]:
                    if e == 'pe' and ent[5] == 'pe' and ent[4]:
                        continue
                    need(ent[5], ent[6])
        return deps, boxes_in, boxes_out

    def _record(self, sk, v, boxes_in, boxes_out):
        for (name, p0, p1, f0, f1) in boxes_out:
            lst = self.acc.setdefault(name, [])
            lst[:] = [en for en in lst if not (p0 <= en[0] and en[1] <= p1 and f0 <= en[2] and en[3] <= f1)]
            lst.append((p0, p1, f0, f1, True, sk, v))
        for (name, p0, p1, f0, f1) in boxes_in:
            lst = self.acc.setdefault(name, [])
            lst[:] = [en for en in lst if not ((not en[4]) and en[5] == sk and p0 <= en[0] and en[1] <= p1 and f0 <= en[2] and en[3] <= f1)]
            lst.append((p0, p1, f0, f1, False, sk, v))

    def _waits(self, e, deps):
        eng = self.engs[e]
        kn = self.known[e]
        for sk, v in deps.items():
            if kn.get(sk, 0) < v:
                eng.wait_ge(self.sems[sk], v)
                kn[sk] = v

    def op(self, e, fn, outs=(), ins=()):
        deps, bi, bo = self._deps(e, outs, ins)
        self._waits(e, deps)
        inst = fn(self.engs[e])
        self.cnt[e] += 1
        inst.then_inc(self.sems[e], 1)
        self._record(e, self.cnt[e], bi, bo)
        self.ninst += 1
        return inst

    def dma(self, e, out, in_, sem=None, **kw):
        if sem is None:
            sem = 'd%d' % self.dma_rr
            self.dma_rr = (self.dma_rr + 1) % self.ndma
        deps, bi, bo = self._deps(e, [out], [in_])
        if self.cnt[sem] > deps.get(sem, 0):
            deps[sem] = self.cnt[sem]
        self._waits(e, deps)
        inst = self.engs[e].dma_start(out=out, in_=in_, **kw)
        self.cnt[sem] += 16
        inst.then_inc(self.sems[sem], 16)
        self._record(sem, self.cnt[sem], bi, bo)
        self.ninst += 1
        return inst

    def finish(self, e='sp'):
        deps = {}
        for sk, v in self.cnt.items():
            if v > 0:
                deps[sk] = v
        self._waits(e, deps)

    def close(self):
        self.es.close()

    def mm(self, out, lhsT, rhs, start=True, stop=True, **kw):
        return self.op('pe', lambda g: g.matmul(out, lhsT, rhs, start=start, stop=stop, **kw), [out], [lhsT, rhs])

    def tr(self, out, in_, ident):
        return self.op('pe', lambda g: g.transpose(out, in_, ident), [out], [in_, ident])

    def tt(self, e, out, a, b, op_):
        return self.op(e, lambda g: g.tensor_tensor(out, a, b, op_), [out], [a, b])

    def ts(self, e, out, a, s1, s2, op0, op1=None):
        ins = [a] + [s for s in (s1, s2) if isinstance(s, bass.AP)]
        if op1 is None:
            return self.op(e, lambda g: g.tensor_scalar(out, a, s1, None, op0), [out], ins)
        return self.op(e, lambda g: g.tensor_scalar(out, a, s1, s2, op0, op1), [out], ins)

    def stt(self, out, a, s, b, op0, op1, e='dve'):
        ins = [a, b] + ([s] if isinstance(s, bass.AP) else [])
        return self.op(e, lambda g: g.scalar_tensor_tensor(out, a, s, b, op0, op1), [out], ins)

    def actf(self, out, a, func, bias=None, scale=None, accum_out=None):
        kw = {}
        ins = [a]
        outs = [out]
        if bias is not None:
            kw['bias'] = bias
            if isinstance(bias, bass.AP):
                ins.append(bias)
        if scale is not None:
            kw['scale'] = scale
            if isinstance(scale, bass.AP):
                ins.append(scale)
        if accum_out is not None:
            kw['accum_out'] = accum_out
            outs.append(accum_out)
        return self.op('act', lambda g: g.activation(out, a, func, **kw), outs, ins)

    def copy(self, e, out, a):
        if e == 'act':
            return self.op(e, lambda g: g.copy(out, a), [out], [a])
        return self.op(e, lambda g: g.tensor_copy(out, a), [out], [a])

    def memset(self, e, out, val):
        return self.op(e, lambda g: g.memset(out, val), [out], [])

    def red(self, out, a, op_=None, axis=None, e='dve'):
        op_ = op_ or ALU.add
        axis = axis or AX.X
        return self.op(e, lambda g: g.tensor_reduce(out, a, axis, op_), [out], [a])


D = 2048
NCORE = 8


def _run(nc, in_maps):
    res = run_bass_kernel_spmd(nc, in_maps, core_ids=list(range(NCORE)))
    return res.results


def _colT(v, n=16):
    return np.ascontiguousarray(v.reshape(n, 128).T)


def _bc(v, p=128):
    return np.ascontiguousarray(np.broadcast_to(v[None, :], (p, v.shape[0])))


def build_L0():
    nc = bass.Bass("TRN2", target_bir_lowering=False)
    vt = nc.dram_tensor("vt", [128, 80], F32, kind="ExternalInput").ap()
    W = nc.dram_tensor("W", [2048, 3072], F32, kind="ExternalInput").ap()
    bias = nc.dram_tensor("bias", [5, 3072], F32, kind="ExternalInput").ap()
    M = nc.dram_tensor("M", [5, 3072], F32, kind="ExternalOutput").ap()
    kb = KB(nc)
    v = kb.sbuf("v", [128, 80])
    sv = kb.sbuf("sv", [128, 80])
    bs = kb.sbuf("bs", [5, 3072])
    o = kb.sbuf("o", [5, 3072])
    wt = [kb.sbuf("wt%d" % i, [128, 16, 512]) for i in range(2)]
    ps = [kb.psum("ps%d" % i, [128, 512]) for i in range(2)]
    kb.dma('sp', v[:], vt)
    kb.dma('sp', bs[:], bias)
    kb.actf(sv[:], v[:], AF.Silu)
    Wv = W.rearrange("(k p) n -> p k n", p=128)
    for n in range(6):
        w = wt[n % 2]
        kb.dma('sp' if n % 2 == 0 else 'act', w[:], Wv[:, :, n * 512:(n + 1) * 512])
        p = ps[n % 2]
        for k in range(16):
            kb.mm(p[0:5, :], sv[:, k * 5:(k + 1) * 5], w[:, k, :], start=(k == 0), stop=(k == 15))
        kb.tt('dve', o[:, n * 512:(n + 1) * 512], p[0:5, :], bs[:, n * 512:(n + 1) * 512], ALU.add)
    kb.dma('sp', M, o[:])
    kb.finish('sp')
    kb.close()
    return nc


def _rms_rstd(kb, rstd, ssq, n, eps):
    kb.ts('dve', rstd, ssq, 1.0 / n, eps, ALU.mult, ALU.add)
    kb.actf(rstd, rstd, AF.Sqrt)
    kb.op('dve', lambda g: g.reciprocal(rstd, rstd), [rstd], [rstd])


def build_L1(NT=17, NCOL=11680):
    nc = bass.Bass("TRN2", target_bir_lowering=False)
    xin = nc.dram_tensor("xin", [NT * 128, D], F32, kind="ExternalInput").ap()
    W = nc.dram_tensor("W", [D, NCOL], F32, kind="ExternalInput").ap()
    sc = nc.dram_tensor("sc", [128, 32], F32, kind="ExternalInput").ap()
    sh = nc.dram_tensor("sh", [128, 32], F32, kind="ExternalInput").ap()
    g = nc.dram_tensor("g", [128, 16], F32, kind="ExternalInput").ap()
    ident = nc.dram_tensor("ident", [128, 128], F32, kind="ExternalInput").ap()
    P = nc.dram_tensor("P", [NT * 128, NCOL], F32, kind="ExternalOutput").ap()
    kb = KB(nc)
    hT = kb.sbuf("hT", [128, 16, NT * 128], BF16)
    idt = kb.sbuf("idt", [128, 128])
    scs = kb.sbuf("scs", [128, 32])
    shs = kb.sbuf("shs", [128, 32])
    gs = kb.sbuf("gs", [128, 16])
    eff = kb.sbuf("eff", [128, 32])
    xt = [kb.sbuf("xt%d" % i, [128, D]) for i in range(2)]
    junk = kb.sbuf("junk", [128, D])
    ssq = kb.sbuf("ssq", [128, 2])
    rstd = kb.sbuf("rstd", [128, 2])
    ps = [kb.psum("ps%d" % i, [128, 512]) for i in range(8)]
    kb.dma('sp', idt[:], ident)
    kb.dma('sp', scs[:], sc)
    kb.dma('sp', shs[:], sh)
    kb.dma('sp', gs[:], g)
    for w in range(2):
        kb.stt(eff[:, w * 16:(w + 1) * 16], scs[:, w * 16:(w + 1) * 16], 1.0, gs[:], ALU.add, ALU.mult)
    for t in range(NT):
        x_ = xt[t % 2]
        c = t % 2
        kb.dma('sp', x_[:], xin[t * 128:(t + 1) * 128, :])
        kb.actf(junk[:], x_[:], AF.Square)
        kb.red(ssq[:, c:c + 1], junk[:])
        _rms_rstd(kb, rstd[:, c:c + 1], ssq[:, c:c + 1], D, 1e-6)
        kb.ts('dve', x_[:], x_[:], rstd[:, c:c + 1], None, ALU.mult)
        w = 0 if t == 0 else 1
        for j in range(16):
            p = ps[(j // 4) % 2]
            sl = p[:, (j % 4) * 128:(j % 4 + 1) * 128]
            kb.tr(sl, x_[:, j * 128:(j + 1) * 128], idt[:])
            kb.ts('dve', hT[:, j, t * 128:(t + 1) * 128], sl, eff[:, w * 16 + j:w * 16 + j + 1],
                  shs[:, w * 16 + j:w * 16 + j + 1], ALU.mult, ALU.add)
    nch = (NCOL + 511) // 512
    wb = [kb.sbuf("wb%d" % i, [128, 16, 512], BF16) for i in range(2)]
    ot = [kb.sbuf("ot%d" % i, [128, 512]) for i in range(4)]
    Wv = W.rearrange("(k p) n -> p k n", p=128)
    cnt = 0
    for c in range(nch):
        c0 = c * 512
        cw = min(512, NCOL - c0)
        w = wb[c % 2]
        kb.dma('pool', w[:, :, 0:cw], Wv[:, :, c0:c0 + cw])
        for t in range(NT):
            p = ps[2 + cnt % 6]
            for k in range(16):
                kb.mm(p[:, 0:cw], hT[:, k, t * 128:(t + 1) * 128], w[:, k, 0:cw], start=(k == 0), stop=(k == 15))
            o = ot[cnt % 4]
            kb.copy('act' if cnt % 2 == 0 else 'dve', o[:, 0:cw], p[:, 0:cw])
            kb.dma('sp' if cnt % 2 == 0 else 'act', P[t * 128:(t + 1) * 128, c0:c0 + cw], o[:, 0:cw])
            cnt += 1
    kb.finish('sp')
    kb.close()
    return nc


T_ALL = 4352
_GS = 6
_GDBG = False
NCH = 68


def build_L2a(NU=15):
    nc = bass.Bass("TRN2", target_bir_lowering=False)
    PJ = nc.dram_tensor("PJ", [NU * 128, T_ALL], F32, kind="ExternalInput").ap()
    CW = nc.dram_tensor("CW", [128, NU * 9], F32, kind="ExternalInput").ap()
    CV = nc.dram_tensor("CV", [NU * 128, T_ALL], F32, kind="ExternalOutput").ap()
    kb = KB(nc)
    cwt = kb.sbuf("cwt", [128, NU * 9])
    xi = [kb.sbuf("xi%d" % i, [128, T_ALL]) for i in range(2)]
    xo = [kb.sbuf("xo%d" % i, [128, T_ALL]) for i in range(2)]
    kb.dma('sp', cwt[:], CW)
    for u in range(NU):
        a = xi[u % 2]
        o = xo[u % 2]
        kb.dma('sp', a[:], PJ[u * 128:(u + 1) * 128, :])
        wv = lambda tap: cwt[:, u * 9 + tap:u * 9 + tap + 1]
        kb.ts('dve', o[:], a[:], wv(4), None, ALU.mult)
        for j in (0, 2):
            dx = j - 1
            c0, c1 = max(0, -dx), 256 - max(0, dx)
            kb.stt(o[:, c0:c1], a[:, c0 + dx:c1 + dx], wv(3 + j), o[:, c0:c1], ALU.mult, ALU.add)
        li = a[:, 256:].rearrange("p (r c) -> p r c", c=64)
        lo = o[:, 256:].rearrange("p (r c) -> p r c", c=64)
        for i in range(3):
            for j in range(3):
                if i == 1 and j == 1:
                    continue
                dy, dx = i - 1, j - 1
                r0, r1 = max(0, -dy), 64 - max(0, dy)
                c0, c1 = max(0, -dx), 64 - max(0, dx)
                kb.stt(lo[:, r0:r1, c0:c1], li[:, r0 + dy:r1 + dy, c0 + dx:c1 + dx], wv(i * 3 + j),
                       lo[:, r0:r1, c0:c1], ALU.mult, ALU.add)
        kb.dma('act', CV[u * 128:(u + 1) * 128, :], o[:])
    kb.finish('sp')
    kb.close()
    return nc


A_DECAY_SCALE = float(np.exp(-0.5))


def build_LR(NCHUNK=NCH):
    T = NCHUNK * 64
    nc = bass.Bass("TRN2", target_bir_lowering=False)
    din = lambda n, s: nc.dram_tensor(n, s, F32, kind="ExternalInput").ap()
    RK = din("RK", [2, T, 768])
    VFM = din("VFM", [128, 6, T])
    VTOK = din("VTOK", [2, T, 384])
    LW = din("LW", [2, 64, T])
    LA = din("LA", [2, 64, T])
    LG = din("LG", [128, T])
    W2 = din("W2", [2, 65, 384])
    A2 = din("A2", [2, 65, 384])
    G2 = din("G2", [128, 384])
    BCS = din("BCS", [128, 5 * 384])
    SEL = din("SEL", [128, 64 * 128])
    I2 = din("I2", [128, 64])
    Z = nc.dram_tensor("Z", [2, T, 384], F32, kind="ExternalOutput").ap()
    G = nc.dram_tensor("G", [T, 384], F32, kind="ExternalOutput").ap()
    DQ = nc.dram_tensor("DQ", [2, T, 1920], F32, kind="Internal").ap()
    kb = KB(nc)
    kb.track_dram("DQ")
    bcs = kb.sbuf("bcs", [128, 5, 6, 64])
    sel = kb.sbuf("sel", [128, 64 * 128])
    i2 = kb.sbuf("i2", [128, 64])
    w2 = kb.sbuf("w2", [65, 2, 384])
    a2 = kb.sbuf("a2", [65, 2, 384])
    g2 = kb.sbuf("g2", [128, 384])
    kb.dma('sp', bcs[:].rearrange("p a h k -> p (a h k)"), BCS)
    kb.dma('sp', sel[:], SEL)
    kb.dma('sp', i2[:], I2)
    kb.dma('sp', g2[:], G2)
    for d in range(2):
        kb.dma('sp', w2[:, d, :], W2[d])
        kb.dma('sp', a2[:, d, :], A2[d])
    kkbc, kabc, rkbc, gnw, gnb = [bcs[:, i] for i in range(5)]
    ps = [kb.psum("ps%d" % i, [128, 512]) for i in range(8)]
    lw = [kb.sbuf("lw%d" % i, [65, 128]) for i in range(2)]
    la = [kb.sbuf("la%d" % i, [65, 128]) for i in range(2)]
    lg = [kb.sbuf("lg%d" % i, [128, 128]) for i in range(2)]
    dq = [kb.sbuf("dq%d" % i, [128, 5, 6, 64]) for i in range(2)]
    kt = [kb.sbuf("kt%d" % i, [128, 6, 64]) for i in range(2)]
    at = kb.sbuf("at", [128, 6, 64])
    t1 = kb.sbuf("t1", [128, 6, 64])
    t2 = kb.sbuf("t2", [128, 6, 64])
    s6 = kb.sbuf("s6", [128, 8])
    gt = [kb.sbuf("gt%d" % i, [128, 384]) for i in range(2)]
    for i in range(2):
        kb.memset('dve', lw[i][:], 1.0)
        kb.memset('dve', la[i][:], 1.0)
    f3 = lambda ap: ap.rearrange("p h k -> p (h k)")
    bc6 = lambda ap: ap.unsqueeze(2).broadcast_to([128, 6, 64])
    it = 0
    for d in range(2):
        for t in range(T // 128):
            b = it % 2
            it += 1
            ts_ = slice(t * 128, (t + 1) * 128)
            q = dq[b]
            k_ = kt[b]
            kb.dma('sp', f3(q[:, 4]), RK[d, ts_, 0:384])
            kb.dma('sp', f3(k_[:]), RK[d, ts_, 384:768])
            kb.dma('act', lw[b][0:64, :], LW[d, :, ts_])
            kb.dma('act', la[b][0:64, :], LA[d, :, ts_])
            kb.actf(lw[b][0:64, :], lw[b][0:64, :], AF.Tanh)
            pw = ps[0]
            pa = ps[1]
            kb.mm(pw[:, 0:384], lw[b][:, :], w2[:, d, :])
            kb.mm(pa[:, 0:384], la[b][:, :], a2[:, d, :])
            kb.actf(f3(t1[:]), pw[:, 0:384], AF.Sigmoid)
            kb.actf(f3(q[:, 1]), f3(t1[:]), AF.Exp, scale=-A_DECAY_SCALE)
            kb.actf(f3(at[:]), pa[:, 0:384], AF.Sigmoid)
            kb.tt('dve', t1[:], k_[:], kkbc, ALU.mult)
            kb.tt('dve', t2[:], t1[:], t1[:], ALU.mult)
            kb.red(s6[:, 0:6], t2[:])
            kb.ts('dve', s6[:, 0:6], s6[:, 0:6], 1e-12, None, ALU.add)
            kb.actf(s6[:, 0:6], s6[:, 0:6], AF.Sqrt)
            kb.op('dve', lambda g: g.reciprocal(s6[:, 0:6], s6[:, 0:6]), [s6[:, 0:6]], [s6[:, 0:6]])
            kb.tt('dve', q[:, 0], t1[:], bc6(s6[:, 0:6]), ALU.mult)
            kb.tt('dve', q[:, 2], q[:, 0], at[:], ALU.mult)
            kb.stt(t2[:], at[:], -1.0, kabc, ALU.add, ALU.mult)
            kb.stt(q[:, 3], t2[:], 1.0, k_[:], ALU.add, ALU.mult)
            kb.dma('sp', DQ[d, ts_, :], q[:].rearrange("p a h k -> p (a h k)"))
            if d == 0:
                kb.dma('act', lg[b][:], LG[:, ts_])
                kb.actf(lg[b][:], lg[b][:], AF.Sigmoid)
                pg = ps[2]
                kb.mm(pg[:, 0:384], lg[b][:], g2[:])
                kb.copy('act', gt[b][:], pg[:, 0:384])
                kb.dma('sp', G[ts_, :], gt[b][:])
    S = kb.sbuf("S", [128, 6, 64])
    tmp = kb.sbuf("tmp", [128, 6, 64])
    tmp2 = kb.sbuf("tmp2", [128, 6, 64])
    sa = kb.sbuf("sa", [128, 6])
    pt = [kb.sbuf("pt%d" % i, [128, 5, 6, 64]) for i in range(2)]
    vv = [kb.sbuf("vv%d" % i, [128, 6, 64]) for i in range(2)]
    vtk = [kb.sbuf("vtk%d" % i, [128, 6, 64]) for i in range(2)]
    ycol = [kb.sbuf("ycol%d" % i, [128, 6, 64]) for i in range(2)]
    yblk = [kb.sbuf("yblk%d" % i, [128, 6, 128]) for i in range(2)]
    yt = kb.sbuf("yt", [128, 6, 64])
    cen = kb.sbuf("cen", [128, 6, 64])
    zt = [kb.sbuf("zt%d" % i, [128, 6, 64]) for i in range(2)]
    st = kb.sbuf("st", [128, 24])
    for i in range(2):
        kb.memset('dve', yblk[i][:], 0.0)
    kb.memset('dve', S[:], 0.0)
    pi = 0
    for c in range(NCHUNK):
        b = c % 2
        cs = slice(c * 64, (c + 1) * 64)
        p_ = pt[b]
        pflat = p_[:].rearrange("p a h k -> p (a h k)")
        kb.dma('sp', pflat[0:64, :], DQ[0, cs, :])
        kb.dma('act', pflat[64:128, :], DQ[1, cs, :])
        kb.dma('sp', vv[b][:], VFM[:, :, cs])
        kb.dma('act', f3(vtk[b][0:64]), VTOK[0, cs, :])
        kb.dma('act', f3(vtk[b][64:128]), VTOK[1, cs, :])
        yc = ycol[b]
        for t in range(64):
            lhsT = sel[:, t * 128:(t + 1) * 128]
            bcl = []
            for j in range(5):
                p = ps[3 + pi % 5]
                pi += 1
                kb.mm(p[:, 0:384], lhsT, f3(p_[:, j]))
                bcl.append(p[:, 0:384].rearrange("p (h k) -> p h k", h=6))
            kkb, wb, kab, kdb, rb = bcl
            kb.tt('dve', tmp[:], S[:], kkb, ALU.mult)
            kb.red(sa[:], tmp[:])
            kb.tt('dve', S[:], S[:], wb, ALU.mult)
            kb.tt('dve', tmp2[:], kab, bc6(sa[:]), ALU.mult)
            kb.tt('dve', S[:], S[:], tmp2[:], ALU.subtract)
            kb.tt('dve', tmp2[:], kdb, bc6(vv[b][:, :, t]), ALU.mult)
            kb.tt('dve', S[:], S[:], tmp2[:], ALU.add)
            kb.tt('dve', tmp[:], S[:], rb, ALU.mult)
            kb.red(yc[:, :, t], tmp[:])
        yb_ = yblk[b]
        kb.copy('act', yb_[0:64, :, 0:64], yc[0:64])
        kb.copy('act', yb_[64:128, :, 64:128], yc[64:128])
        py = ps[0]
        for h in range(6):
            kb.mm(py[:, h * 64:(h + 1) * 64], yb_[:, h, :], i2[:])
        kb.copy('act', f3(yt[:]), py[:, 0:384])
        kb.red(st[:, 0:6], yt[:])
        kb.ts('dve', st[:, 0:6], st[:, 0:6], -1.0 / 64, None, ALU.mult)
        kb.tt('dve', cen[:], yt[:], bc6(st[:, 0:6]), ALU.add)
        kb.tt('dve', yt[:], cen[:], cen[:], ALU.mult)
        kb.red(st[:, 6:12], yt[:])
        _rms_rstd(kb, st[:, 6:12], st[:, 6:12], 64, 64e-5)
        kb.tt('dve', cen[:], cen[:], bc6(st[:, 6:12]), ALU.mult)
        kb.tt('dve', cen[:], cen[:], gnw, ALU.mult)
        kb.tt('dve', cen[:], cen[:], gnb, ALU.add)
        kb.tt('dve', yt[:], p_[:, 4], p_[:, 3], ALU.mult)
        kb.tt('dve', yt[:], yt[:], rkbc, ALU.mult)
        kb.red(st[:, 12:18], yt[:])
        z = zt[b]
        kb.tt('dve', z[:], vtk[b][:], bc6(st[:, 12:18]), ALU.mult)
        kb.tt('dve', z[:], z[:], cen[:], ALU.add)
        kb.dma('sp', Z[0, cs, :], f3(z[0:64]))
        kb.dma('act', Z[1, cs, :], f3(z[64:128]))
    kb.finish('sp')
    kb.close()
    return nc


def build_LG(NCHUNK=NCH):
    T = NCHUNK * 64
    nc = bass.Bass("TRN2", target_bir_lowering=False)
    din = lambda n, s: nc.dram_tensor(n, s, F32, kind="ExternalInput").ap()
    QKF = din("QKF", [4, 2, 96, T])
    KT = din("KT", [4, T, 96])
    VT = din("VT", [4, T, 192])
    DA = din("DA", [2, 16, T])
    A2 = din("A2", [2, 17, 192])
    TRI = din("TRI", [64, 128])
    O = nc.dram_tensor("O", [2, T, 384], F32, kind="ExternalOutput").ap()
    kb = KB(nc)
    tri = kb.sbuf("tri", [64, 128])
    a2 = kb.sbuf("a2", [17, 2, 192])
    kb.dma('sp', tri[:], TRI)
    for d in range(2):
        kb.dma('sp', a2[:, d, :], A2[d])
    tinc = tri[:, 0:64]
    taft = tri[:, 64:128]
    ps = [kb.psum("ps%d" % i, [128, 512]) for i in range(8)]
    U = []
    for u in range(4):
        b = {}
        b['S'] = kb.sbuf("S%d" % u, [96, 192])
        kb.memset('dve', b['S'][:], 0.0)
        for nm, shp in [("da", [17, 64]), ("qk", [96, 2, 64]), ("kt", [64, 96]), ("vt", [64, 192]), ("g", [64, 96]),
                        ("e1", [64, 96]), ("kdec", [64, 96]), ("cg", [96, 64]), ("sc", [96, 4]), ("eq", [96, 64]),
                        ("qp", [96, 64]), ("kp", [96, 64]), ("qd", [96, 64]), ("scm", [64, 64]), ("o", [64, 192])]:
            b[nm] = kb.sbuf("%s%d" % (nm, u), shp)
        kb.memset('dve', b['da'][:], 1.0)
        U.append(b)
    scale = 96.0 ** -0.5
    for c in range(NCHUNK):
        cs = slice(c * 64, (c + 1) * 64)
        for u in range(4):
            d, hh = u // 2, u % 2
            b = U[u]
            p0 = ps[(2 * u) % 8]
            p1 = ps[(2 * u + 1) % 8]
            kb.dma('sp', b['da'][0:16, :], DA[d, :, cs])
            kb.dma('sp', b['qk'][:], QKF[u, :, :, cs].rearrange("a k t -> k a t"))
            kb.dma('act', b['kt'][:], KT[u, cs, :])
            kb.dma('act', b['vt'][:], VT[u, cs, :])
            kb.mm(p0[0:64, 0:96], b['da'][:, :], a2[:, d, hh * 96:(hh + 1) * 96])
            kb.actf(b['g'][:], p0[0:64, 0:96], AF.Exp, scale=-1.0)
            kb.actf(b['g'][:], b['g'][:], AF.Ln, bias=1.0)
            kb.ts('dve', b['g'][:], b['g'][:], -1.0 / 16.0, None, ALU.mult)
            kb.mm(p0[0:64, 128:224], taft, b['g'][:])
            kb.mm(p0[0:96, 256:320], b['g'][:], tinc)
            kb.actf(b['e1'][:], p0[0:64, 128:224], AF.Exp)
            kb.tt('dve', b['kdec'][:], b['kt'][:], b['e1'][:], ALU.mult)
            kb.copy('act', b['cg'][:], p0[0:96, 256:320])
            sc = b['sc']
            kb.ts('dve', sc[:, 0:1], b['cg'][:, 31:32], -1.0, None, ALU.mult)
            kb.actf(sc[:, 1:2], b['cg'][:, 63:64], AF.Exp)
            kb.actf(b['eq'][:], b['cg'][:], AF.Exp, bias=sc[:, 0:1])
            kb.stt(b['qp'][:], b['eq'][:], scale, b['qk'][:, 0, :], ALU.mult, ALU.mult)
            kb.actf(b['eq'][:], b['cg'][:], AF.Exp, scale=-1.0, bias=b['cg'][:, 31:32])
            kb.tt('dve', b['kp'][:], b['eq'][:], b['qk'][:, 1, :], ALU.mult)
            kb.actf(b['eq'][:], b['cg'][:], AF.Exp)
            kb.stt(b['qd'][:], b['eq'][:], scale, b['qk'][:, 0, :], ALU.mult, ALU.mult)
            kb.mm(p1[0:96, 0:192], b['kdec'][:], b['vt'][:])
            kb.mm(p0[0:64, 384:448], b['kp'][:], b['qp'][:])
            kb.tt('dve', b['scm'][:], p0[0:64, 384:448], tinc, ALU.mult)
            kb.mm(p1[0:64, 256:448], b['qd'][:], b['S'][:], start=True, stop=False)
            kb.mm(p1[0:64, 256:448], b['scm'][:], b['vt'][:], start=False, stop=True)
            kb.copy('act', b['o'][:], p1[0:64, 256:448])
            kb.dma('sp', O[d, cs, hh * 192:(hh + 1) * 192], b['o'][:])
            kb.stt(b['S'][:], b['S'][:], sc[:, 1:2], p1[0:96, 0:192], ALU.mult, ALU.add)
    kb.finish('sp')
    kb.close()
    return nc


TWO_PI = float(2 * np.pi)


def _sin_reduced(kb, out, ang, tmp, tmpi, mask):
    kb.ts('dve', tmp, ang, 1.0 / TWO_PI, None, ALU.mult)
    kb.copy('dve', tmpi, tmp)
    kb.copy('dve', tmp, tmpi)
    kb.stt(out, tmp, -TWO_PI, ang, ALU.mult, ALU.add)
    kb.ts('dve', mask, out, float(np.pi), -TWO_PI, ALU.is_gt, ALU.mult)
    kb.tt('dve', out, out, mask, ALU.add)
    kb.ts('dve', mask, out, -float(np.pi), TWO_PI, ALU.is_lt, ALU.mult)
    kb.tt('dve', out, out, mask, ALU.add)
    kb.actf(out, out, AF.Sin)


def build_LS(NCK=17):
    CK = 256
    T = NCK * CK
    nc = bass.Bass("TRN2", target_bir_lowering=False)
    din = lambda n, s: nc.dram_tensor(n, s, F32, kind="ExternalInput").ap()
    UF = din("UF", [2, 256, T])
    LRI = din("LRI", [128, 48])
    BT = din("BT", [32, 2 * 8 * 128])
    CT = din("CT", [128, 2 * 8 * 32])
    YC = nc.dram_tensor("YC", [2, 256, T], F32, kind="ExternalOutput").ap()
    kb = KB(nc)
    lri = kb.sbuf("lri", [128, 48])
    bt = kb.sbuf("bt", [32, 2, 8, 128])
    ct = kb.sbuf("ct", [128, 2, 8, 32])
    kb.dma('sp', lri[:], LRI)
    kb.dma('sp', bt[:].rearrange("p a j m -> p (a j m)"), BT)
    kb.dma('sp', ct[:].rearrange("p a j m -> p (a j m)"), CT)
    kb.actf(ct[:, 1], ct[:, 1], AF.Copy, scale=-1.0)
    lr, li, ldt = lri[:, 0:16], lri[:, 16:32], lri[:, 32:48]
    W = kb.sbuf("W", [128, 16, 16])
    w = lambda i: W[:, i, :]
    wi = kb.sbuf("wi", [128, 16], mybir.dt.int32)
    dt, mag, ang, sn, cs_, tmp, msk, ar, ai, am1, rden, fr, fi = [w(i) for i in range(13)]
    kb.actf(dt, ldt, AF.Exp)
    kb.tt('dve', tmp, lr, dt, ALU.mult)
    kb.actf(mag, tmp, AF.Exp)
    kb.tt('dve', ang, li, dt, ALU.mult)
    _sin_reduced(kb, sn, ang, tmp, wi[:], msk)
    kb.ts('dve', ang, ang, float(np.pi / 2), None, ALU.add)
    _sin_reduced(kb, cs_, ang, tmp, wi[:], msk)
    kb.tt('dve', ar, mag, cs_, ALU.mult)
    kb.tt('dve', ai, mag, sn, ALU.mult)
    kb.ts('dve', am1, ar, -1.0, None, ALU.add)
    kb.tt('dve', tmp, lr, lr, ALU.mult)
    kb.tt('dve', msk, li, li, ALU.mult)
    kb.tt('dve', tmp, tmp, msk, ALU.add)
    kb.op('dve', lambda g: g.reciprocal(rden, tmp), [rden], [tmp])
    kb.tt('dve', fr, am1, lr, ALU.mult)
    kb.tt('dve', tmp, ai, li, ALU.mult)
    kb.tt('dve', fr, fr, tmp, ALU.add)
    kb.tt('dve', fr, fr, rden, ALU.mult)
    kb.tt('dve', fi, ai, lr, ALU.mult)
    kb.tt('dve', tmp, am1, li, ALU.mult)
    kb.tt('dve', fi, fi, tmp, ALU.subtract)
    kb.tt('dve', fi, fi, rden, ALU.mult)
    ps = [kb.psum("ps%d" % i, [128, 512]) for i in range(8)]
    uf = [kb.sbuf("uf%d" % i, [32, 2, 8, CK]) for i in range(2)]
    bu = [kb.sbuf("bu%d" % i, [128, 2, 16, CK]) for i in range(2)]
    X = [kb.sbuf("X%d" % i, [128, 2, 16, CK]) for i in range(2)]
    t4 = kb.sbuf("t4", [128, 4, 16])
    tm = kb.sbuf("tm", [128, 2, CK])
    yo = [kb.sbuf("yo%d" % i, [32, CK]) for i in range(4)]
    x0 = kb.sbuf("x0", [128, 2, 16])
    kb.memset('dve', x0[:], 0.0)
    pi = 0
    yi = 0
    for c in range(NCK):
        b = c % 2
        cs = slice(c * CK, (c + 1) * CK)
        for d in range(2):
            kb.dma('sp' if d == 0 else 'act', uf[b][:, d], UF[d, :, cs].rearrange("(j c) t -> c j t", c=32))
        for d in range(2):
            for j in range(8):
                col = d * 8 + j
                p = ps[pi % 4]
                pi += 1
                kb.mm(p[:, 0:CK], bt[:, 0, j, :], uf[b][:, d, j, :])
                kb.mm(p[:, CK:2 * CK], bt[:, 1, j, :], uf[b][:, d, j, :])
                pr, pim = p[:, 0:CK], p[:, CK:2 * CK]
                kb.ts('dve', tm[:, 0], pim, fi[:, col:col + 1], None, ALU.mult)
                kb.stt(bu[b][:, 0, col, :], pr, fr[:, col:col + 1], tm[:, 0], ALU.mult, ALU.subtract)
                kb.ts('dve', tm[:, 1], pr, fi[:, col:col + 1], None, ALU.mult)
                kb.stt(bu[b][:, 1, col, :], pim, fr[:, col:col + 1], tm[:, 1], ALU.mult, ALU.add)
        Xc = X[b]
        for t in range(CK):
            if t == 0:
                xr, xi = (x0[:, 0], x0[:, 1]) if c == 0 else (X[1 - b][:, 0, :, CK - 1], X[1 - b][:, 1, :, CK - 1])
            else:
                xr, xi = Xc[:, 0, :, t - 1], Xc[:, 1, :, t - 1]
            kb.tt('dve', t4[:, 0], ar, xr, ALU.mult)
            kb.tt('dve', t4[:, 1], ar, xi, ALU.mult)
            kb.tt('dve', t4[:, 2], ai, xi, ALU.mult)
            kb.tt('dve', t4[:, 3], ai, xr, ALU.mult)
            kb.tt('dve', t4[:, 0], t4[:, 0], t4[:, 2], ALU.subtract)
            kb.tt('dve', t4[:, 1], t4[:, 1], t4[:, 3], ALU.add)
            kb.tt('dve', Xc[:, 0, :, t], t4[:, 0], bu[b][:, 0, :, t], ALU.add)
            kb.tt('dve', Xc[:, 1, :, t], t4[:, 1], bu[b][:, 1, :, t], ALU.add)
        for d in range(2):
            for j in range(8):
                col = d * 8 + j
                p = ps[4 + pi % 4]
                pi += 1
                kb.mm(p[0:32, 0:CK], ct[:, 0, j, :], Xc[:, 0, col, :], start=True, stop=False)
                kb.mm(p[0:32, 0:CK], ct[:, 1, j, :], Xc[:, 1, col, :], start=False, stop=True)
                y = yo[yi % 4]
                yi += 1
                kb.copy('act', y[:], p[0:32, 0:CK])
                kb.dma('sp', YC[d, j * 32:(j + 1) * 32, cs], y[:])
    kb.finish('sp')
    kb.close()
    return nc


def build_L3a(NT=17):
    NTOK = NT * 128
    nc = bass.Bass("TRN2", target_bir_lowering=False)
    din = lambda n, s: nc.dram_tensor(n, s, F32, kind="ExternalInput").ap()
    XIN = din("XIN", [NTOK, D])
    ZA = din("ZA", [2, NTOK, 768])
    GA = din("GA", [NTOK, 768])
    OB = din("OB", [2, NTOK, 768])
    OG = din("OG", [NTOK, 768])
    YCF = din("YCF", [2, 512, NTOK])
    UF = din("UF", [512, NTOK])
    BG = din("BG", [NTOK, 6144])
    MODB = din("MODB", [2, 3, 128, D])
    G2B = din("G2B", [128, D])
    NGB = din("NGB", [128, 768])
    DSK = din("DSK", [128, 4])
    PA = din("PA", [768, D])
    PB = din("PB", [768, D])
    PC = din("PC", [512, D])
    WO = din("WO", [D, D])
    GLU = din("GLU", [512, 1024])
    ident = din("ident", [128, 128])
    XMID = nc.dram_tensor("XMID", [NTOK, D], F32, kind="ExternalOutput").ap()
    H2 = nc.dram_tensor("H2", [NTOK, D], F32, kind="ExternalOutput").ap()
    M = nc.dram_tensor("M", [NTOK, D], F32, kind="Internal").ap()
    kb = KB(nc)
    kb.track_dram("M")
    idt = kb.sbuf("idt", [128, 128])
    wbig = kb.sbuf("wbig", [128, 16 * D], BF16)
    gluw = kb.sbuf("gluw", [128, 4, 1024], BF16)
    modb = kb.sbuf("modb", [128, 3, D])
    g2b = kb.sbuf("g2b", [128, D])
    ngb = kb.sbuf("ngb", [128, 768])
    dsk = kb.sbuf("dsk", [128, 4])
    kb.dma('sp', idt[:], ident)
    kb.dma('sp', g2b[:], G2B)
    kb.dma('sp', ngb[:], NGB)
    kb.dma('sp', dsk[:], DSK)
    pa = wbig[:, 0:6 * D].rearrange("p (k n) -> p k n", k=6)
    pb = wbig[:, 6 * D:12 * D].rearrange("p (k n) -> p k n", k=6)
    pc = wbig[:, 12 * D:16 * D].rearrange("p (k n) -> p k n", k=4)
    kb.dma('pool', pa, PA.rearrange("(k p) n -> p k n", p=128))
    kb.dma('pool', pb, PB.rearrange("(k p) n -> p k n", p=128))
    kb.dma('pool', pc, PC.rearrange("(k p) n -> p k n", p=128))
    kb.dma('pool', gluw[:], GLU.rearrange("(k p) n -> p k n", p=128))
    ps = [kb.psum("ps%d" % i, [128, 512]) for i in range(8)]
    a0 = kb.sbuf("a0", [128, 768])
    a1 = kb.sbuf("a1", [128, 768])
    a2_ = kb.sbuf("a2_", [128, 768])
    yaT = kb.sbuf("yaT", [128, 6, 128], BF16)
    ybT = kb.sbuf("ybT", [128, 6, 128], BF16)
    ycT = kb.sbuf("ycT", [128, 4, 128], BF16)
    s8 = kb.sbuf("s8", [128, 8])
    yf = kb.sbuf("yf", [128, 3, 4, 128])
    gT = kb.sbuf("gT", [128, 4, 128], BF16)
    gt1 = kb.sbuf("gt1", [128, 4, 128])
    gt2 = kb.sbuf("gt2", [128, 4, 128])
    yc = kb.sbuf("yc", [128, 512])
    bg = kb.sbuf("bg", [128, 3, 512])
    m = kb.sbuf("m", [128, D])
    tq = kb.sbuf("tq", [128, 512])

    def transposes(dstT, src, n, pbase):
        for j in range(n):
            p = ps[pbase + (j // 4) % 2]
            sl = p[:, (j % 4) * 128:(j % 4 + 1) * 128]
            kb.tr(sl, src[:, j * 128:(j + 1) * 128], idt[:])
            kb.copy('act' if j % 2 else 'dve', dstT[:, j, :], sl)

    for t in range(NT):
        ts_ = slice(t * 128, (t + 1) * 128)
        kb.dma('sp', a0[:], ZA[0, ts_, :])
        kb.dma('act', a1[:], ZA[1, ts_, :])
        kb.dma('sp', a2_[:], GA[ts_, :])
        kb.tt('dve', a0[:], a0[:], a1[:], ALU.add)
        kb.tt('dve', a0[:], a0[:], a2_[:], ALU.mult)
        transposes(yaT, a0, 6, 0)
        kb.dma('sp', a0[:], OB[0, ts_, :])
        kb.dma('act', a1[:], OB[1, ts_, :])
        kb.dma('sp', a2_[:], OG[ts_, :])
        kb.tt('dve', a0[:], a0[:], a1[:], ALU.add)
        kb.tt('dve', a1[:], a0[:], a0[:], ALU.mult)
        kb.red(s8[:, 0:4], a1[:].rearrange("p (h v) -> p h v", h=4))
        _rms_rstd(kb, s8[:, 0:4], s8[:, 0:4], 192, 1e-6)
        kb.tt('dve', a0[:].rearrange("p (h v) -> p h v", h=4), a0[:].rearrange("p (h v) -> p h v", h=4),
              s8[:, 0:4].unsqueeze(2).broadcast_to([128, 4, 192]), ALU.mult)
        kb.tt('dve', a0[:], a0[:], ngb[:], ALU.mult)
        kb.actf(a2_[:], a2_[:], AF.Silu)
        kb.tt('dve', a0[:], a0[:], a2_[:], ALU.mult)
        transposes(ybT, a0, 6, 0)
        kb.dma('sp', yf[:, 0], YCF[0, :, ts_].rearrange("(j p) t -> p j t", p=128))
        kb.dma('act', yf[:, 1], YCF[1, :, ts_].rearrange("(j p) t -> p j t", p=128))
        kb.dma('sp', yf[:, 2], UF[:, ts_].rearrange("(j p) t -> p j t", p=128))
        kb.tt('dve', gt1[:], yf[:, 0], yf[:, 1], ALU.add)
        for j in range(4):
            kb.stt(gt1[:, j], yf[:, 2, j], dsk[:, j:j + 1], gt1[:, j], ALU.mult, ALU.add)
        kb.tt('dve', gt2[:], gt1[:], gt1[:], ALU.mult)
        kb.ts('dve', gt2[:], gt2[:], 0.044715, 1.0, ALU.mult, ALU.add)
        kb.tt('dve', gt2[:], gt2[:], gt1[:], ALU.mult)
        kb.actf(gt2[:], gt2[:], AF.Tanh, scale=0.7978845608028654)
        kb.ts('dve', gt2[:], gt2[:], 1.0, 0.5, ALU.add, ALU.mult)
        kb.tt('dve', gT[:], gt2[:], gt1[:], ALU.mult)
        for n in range(2):
            for j in range(4):
                kb.mm(ps[2 + n][:, :], gT[:, j, :], gluw[:, j, n * 512:(n + 1) * 512], start=(j == 0), stop=(j == 3))
        kb.actf(tq[:], ps[3][:, :], AF.Sigmoid)
        kb.tt('dve', yc[:], ps[2][:, :], tq[:], ALU.mult)
        transposes(ycT, yc, 4, 0)
        for n in range(4):
            ns = slice(n * 512, (n + 1) * 512)
            for br in range(3):
                kb.dma('sp' if br != 1 else 'act', bg[:, br, :], BG[ts_, br * D + n * 512:br * D + (n + 1) * 512])
            kb.actf(bg[:].rearrange("p a n -> p (a n)"), bg[:].rearrange("p a n -> p (a n)"), AF.Sigmoid)
            for k in range(6):
                kb.mm(ps[4][:, :], yaT[:, k, :], pa[:, k, ns], start=(k == 0), stop=(k == 5))
            for k in range(6):
                kb.mm(ps[5][:, :], ybT[:, k, :], pb[:, k, ns], start=(k == 0), stop=(k == 5))
            for k in range(4):
                kb.mm(ps[6][:, :], ycT[:, k, :], pc[:, k, ns], start=(k == 0), stop=(k == 3))
            kb.tt('dve', m[:, ns], ps[4][:, :], bg[:, 0, :], ALU.mult)
            kb.tt('dve', tq[:], ps[5][:, :], bg[:, 1, :], ALU.mult)
            kb.tt('dve', m[:, ns], m[:, ns], tq[:], ALU.add)
            kb.tt('dve', tq[:], ps[6][:, :], bg[:, 2, :], ALU.mult)
            kb.tt('dve', m[:, ns], m[:, ns], tq[:], ALU.add)
        kb.dma('sp', M[ts_, :], m[:])
    wo = wbig[:].rearrange("p (k n) -> p k n", k=16)
    kb.dma('pool', wo, WO.rearrange("(k p) n -> p k n", p=128))
    mT = kb.sbuf("mT", [128, 16, 128], BF16)
    xr = kb.sbuf("xr", [128, D])
    h2 = kb.sbuf("h2", [128, D])
    for t in range(NT):
        ts_ = slice(t * 128, (t + 1) * 128)
        if t <= 1:
            w = 0 if t == 0 else 1
            kb.dma('sp', modb[:].rearrange("p a n -> p (a n)"), MODB[w].rearrange("a p n -> p a n"))
            kb.stt(modb[:, 1], modb[:, 1], 1.0, g2b[:], ALU.add, ALU.mult)
        kb.dma('sp', m[:], M[ts_, :])
        kb.dma('act', xr[:], XIN[ts_, :])
        transposes(mT, m, 16, 0)
        for n in range(4):
            ns = slice(n * 512, (n + 1) * 512)
            p = ps[2 + n]
            for k in range(16):
                kb.mm(p[:, :], mT[:, k, :], wo[:, k, ns], start=(k == 0), stop=(k == 15))
            kb.tt('dve', h2[:, ns], p[:, :], modb[:, 0, ns], ALU.mult)
            kb.tt('dve', xr[:, ns], xr[:, ns], h2[:, ns], ALU.add)
        kb.dma('sp', XMID[ts_, :], xr[:])
        kb.actf(h2[:], xr[:], AF.Square)
        kb.red(s8[:, 4:5], h2[:])
        _rms_rstd(kb, s8[:, 4:5], s8[:, 4:5], D, 1e-6)
        kb.ts('dve', h2[:], xr[:], s8[:, 4:5], None, ALU.mult)
        kb.tt('dve', h2[:], h2[:], modb[:, 1], ALU.mult)
        kb.tt('dve', h2[:], h2[:], modb[:, 2], ALU.add)
        kb.dma('act', H2[ts_, :], h2[:])
    kb.finish('sp')
    kb.close()
    return nc


def build_L3b(NT=17, last=False):
    NTOK = NT * 128
    nc = bass.Bass("TRN2", target_bir_lowering=False)
    din = lambda n, s: nc.dram_tensor(n, s, F32, kind="ExternalInput").ap()
    H2T = din("H2T", [D, NTOK])
    XMID = din("XMID", [NTOK, D])
    GA2B = din("GA2B", [2, 128, D])
    RW = din("RW", [D, 16])
    RBB = din("RBB", [128, 16])
    W1 = din("W1", [16, D, 1024])
    W3 = din("W3", [16, D, 1024])
    W2 = din("W2", [16, 1024, D])
    FGB = din("FGB", [128, D])
    OUT = nc.dram_tensor("OUT", [NTOK, D], F32, kind="ExternalOutput").ap()
    GDBG = nc.dram_tensor("GDBG", [128, NT * 16], F32, kind="ExternalOutput").ap() if _GDBG else None
    kb = KB(nc)
    h2T = kb.sbuf("h2T", [128, 16, NTOK], BF16)
    H2v = H2T.rearrange("(k p) t -> p k t", p=128)
    for k in range(16):
        kb.dma('pool', h2T[:, k, :], H2v[:, k, :])
    rw = kb.sbuf("rw", [128, 16, 16])
    rbb = kb.sbuf("rbb", [128, 16])
    fgb = kb.sbuf("fgb", [128, D])
    ga2 = kb.sbuf("ga2", [128, D])
    kb.dma('sp', rw[:], RW.rearrange("(k p) e -> p k e", p=128))
    kb.dma('sp', rbb[:], RBB)
    kb.dma('sp', fgb[:], FGB)
    ps = [kb.psum("ps%d" % i, [128, 512]) for i in range(8)]
    gate = kb.sbuf("gate", [128, NT, 16])
    hf = [kb.sbuf("hf0", [128, 16, 128])] * 2
    R = kb.sbuf("R", [128, 8, 16])
    r4 = kb.sbuf("r4", [128, 8, 4])
    r1 = kb.sbuf("r1", [128, 8])
    g44 = lambda ap: ap.rearrange("p (g e) -> p g e", g=4)
    b44 = lambda ap: ap.unsqueeze(2).broadcast_to([128, 4, 4])
    for t in range(NT):
        h = hf[t % 2]
        kb.dma('sp' if t % 2 == 0 else 'act', h[:], H2v[:, :, t * 128:(t + 1) * 128])
        for k in range(16):
            kb.mm(ps[0][:, 0:16], h[:, k, :], rw[:, k, :], start=(k == 0), stop=(k == 15))
        sc, bi, mk, sel1, mk2, sel2 = [R[:, i] for i in range(6)]
        gs, t4_, ing, pen = [r4[:, i] for i in range(4)]
        kb.actf(sc, ps[0][:, 0:16], AF.Sigmoid)
        kb.tt('dve', bi, sc, rbb[:], ALU.add)
        bg_ = g44(bi)
        first = True
        for i in range(4):
            for j in range(i + 1, 4):
                if first:
                    kb.tt('dve', gs, bg_[:, :, i], bg_[:, :, j], ALU.add)
                    first = False
                else:
                    kb.tt('dve', t4_, bg_[:, :, i], bg_[:, :, j], ALU.add)
                    kb.tt('dve', gs, gs, t4_, ALU.max)
        kb.red(r1[:, 0:1], gs, ALU.max)
        kb.ts('dve', ing, gs, r1[:, 0:1], None, ALU.is_ge)
        kb.ts('dve', pen, ing, 1.0, 1e9, ALU.subtract, ALU.mult)
        kb.tt('dve', g44(mk), bg_, b44(ing), ALU.mult)
        kb.tt('dve', g44(mk), g44(mk), b44(pen), ALU.add)
        kb.red(r1[:, 1:2], mk, ALU.max)
        kb.ts('dve', sel1, mk, r1[:, 1:2], None, ALU.is_ge)
        kb.stt(mk2, sel1, -1e9, mk, ALU.mult, ALU.add)
        kb.red(r1[:, 2:3], mk2, ALU.max)
        kb.ts('dve', sel2, mk2, r1[:, 2:3], None, ALU.is_ge)
        kb.tt('dve', sel1, sel1, sel2, ALU.add)
        kb.tt('dve', sel1, sel1, sc, ALU.mult)
        kb.red(r1[:, 3:4], sel1)
        kb.op('dve', lambda g: g.reciprocal(r1[:, 4:5], r1[:, 3:4]), [r1[:, 4:5]], [r1[:, 3:4]])
        kb.ts('dve', gate[:, t, :], sel1, r1[:, 4:5], None, ALU.mult)
    if _GDBG:
        kb.dma('sp', GDBG, gate[:].rearrange("p t e -> p (t e)"))
    GS = _GS
    groups = [list(range(i, min(NT, i + GS))) for i in range(0, NT, GS)]
    acc = kb.sbuf("acc", [128, GS, D])
    wq = [dict(w1=kb.sbuf("w1q%d" % i, [128, 16, 256], BF16), w3=kb.sbuf("w3q%d" % i, [128, 16, 256], BF16),
               w2=kb.sbuf("w2q%d" % i, [128, 2, D], BF16)) for i in range(2)]
    s1 = kb.sbuf("s1", [128, 512])
    heT = [kb.sbuf("heT%d" % i, [128, 2, 512], BF16) for i in range(2)]
    xm = kb.sbuf("xm", [128, D])
    junk = hf[0][:].rearrange("p k t -> p (k t)")
    cw = 0
    cpo = 0
    che = 0
    for tiles in groups:
        if not tiles:
            continue
        chunks = [tiles[i:i + 4] for i in range(0, len(tiles), 4)]
        for e in range(16):
            for q in range(4):
                wb = wq[cw % 2]
                cw += 1
                fs = slice(q * 256, (q + 1) * 256)
                kb.dma('pool', wb['w1'][:], W1[e].rearrange("(k p) f -> p k f", p=128)[:, :, fs])
                kb.dma('pool', wb['w3'][:], W3[e].rearrange("(k p) f -> p k f", p=128)[:, :, fs])
                kb.dma('pool', wb['w2'][:], W2[e, fs, :].rearrange("(f p) n -> p f n", p=128))
                for ch in chunks:
                    tok0 = ch[0] * 128
                    ntok = len(ch) * 128
                    he = heT[che % 2]
                    che += 1
                    for f in range(2):
                        p1 = ps[1 + f]
                        p3 = ps[3 + f]
                        for k in range(16):
                            kb.mm(p1[:, 0:ntok], wb['w1'][:, k, f * 128:(f + 1) * 128], h2T[:, k, tok0:tok0 + ntok],
                                  start=(k == 0), stop=(k == 15))
                        for k in range(16):
                            kb.mm(p3[:, 0:ntok], wb['w3'][:, k, f * 128:(f + 1) * 128], h2T[:, k, tok0:tok0 + ntok],
                                  start=(k == 0), stop=(k == 15))
                        kb.actf(s1[:, 0:ntok], p1[:, 0:ntok], AF.Silu)
                        kb.tt('dve', he[:, f, 0:ntok], s1[:, 0:ntok], p3[:, 0:ntok], ALU.mult)
                    for ti, t in enumerate(ch):
                        for n in range(4):
                            po = ps[5 + cpo % 3]
                            cpo += 1
                            for f in range(2):
                                kb.mm(po[:, :], he[:, f, ti * 128:(ti + 1) * 128], wb['w2'][:, f, n * 512:(n + 1) * 512],
                                      start=(f == 0), stop=(f == 1))
                            a = acc[:, t - tiles[0], n * 512:(n + 1) * 512]
                            if e == 0 and q == 0:
                                kb.ts('dve', a, po[:, :], gate[:, t, e:e + 1], None, ALU.mult)
                            else:
                                kb.stt(a, po[:, :], gate[:, t, e:e + 1], a, ALU.mult, ALU.add)
        for t in tiles:
            ts_ = slice(t * 128, (t + 1) * 128)
            if t <= 1:
                kb.dma('sp', ga2[:], GA2B[0 if t == 0 else 1])
            a = acc[:, t - tiles[0], :]
            kb.dma('sp', xm[:], XMID[ts_, :])
            kb.tt('dve', a, a, ga2[:], ALU.mult)
            kb.tt('dve', xm[:], xm[:], a, ALU.add)
            if last:
                kb.actf(junk, xm[:], AF.Square)
                kb.red(r1[:, 5:6], junk)
                _rms_rstd(kb, r1[:, 5:6], r1[:, 5:6], D, 1e-6)
                kb.ts('dve', xm[:], xm[:], r1[:, 5:6], None, ALU.mult)
                kb.tt('dve', xm[:], xm[:], fgb[:], ALU.mult)
            kb.dma('act', OUT[ts_, :], xm[:])
    kb.finish('sp')
    kb.close()
    return nc


def _s5_lay(a):
    return np.ascontiguousarray(a.reshape(2, 8, 2, 64).transpose(2, 3, 0, 1).reshape(128, 16))


def kernel(x, c, ctx, c_ctx, mod_w, mod_b, norm1_g, norm2_g, w_in, conv_w,
           rk_w0, rk_w2, rk_a0, rk_a2, rk_g2, rk_kk, rk_ka, rk_rk, rk_gn_w, rk_gn_b,
           gla_a2, gla_ab, gla_norm_g,
           s5_lam_re, s5_lam_im, s5_log_dt, s5_b_re, s5_b_im, s5_c_re, s5_c_im, s5_d, s5_glu_w,
           proj_a, proj_b, proj_c, w_out, router_w, router_bias, exp_w1, exp_w3, exp_w2, final_g):
    f32 = np.float32
    A = lambda v: np.ascontiguousarray(np.asarray(v, dtype=f32))
    x = A(x).copy()
    ctx = A(ctx).copy()
    T = T_ALL
    ident = np.eye(128, dtype=f32)
    V = np.concatenate([A(c), A(c_ctx)[None]], 0)
    vt = A(V.reshape(5, 16, 128).transpose(2, 1, 0).reshape(128, 80))
    ims = []
    for core in range(8):
        l, q = core // 4, core % 4
        ims.append({"vt": vt, "W": A(mod_w[l][:, q * 3072:(q + 1) * 3072]),
                    "bias": A(np.repeat(mod_b[l][None, q * 3072:(q + 1) * 3072], 5, 0))})
    res = _run(build_L0(), ims)
    mod = [np.concatenate([res[l * 4 + q]["M"] for q in range(4)], 1) for l in range(2)]
    idx = [np.arange(T), np.concatenate([255 - np.arange(256), 4607 - np.arange(256, T)])]
    SEL = np.zeros((128, 64, 128), f32)
    for t in range(64):
        SEL[t, t, 0:64] = 1
        SEL[64 + t, t, 64:128] = 1
    SEL = SEL.reshape(128, -1)
    I2 = np.concatenate([np.eye(64), np.eye(64)], 0).astype(f32)
    TRI = np.concatenate([np.triu(np.ones((64, 64))), np.tril(np.ones((64, 64)), -1)], 1).astype(f32)
    nc1, nc2a, ncR, ncG, ncS, nc3a = build_L1(), build_L2a(), build_LR(), build_LG(), build_LS(), build_L3a()
    toks = [np.concatenate([np.arange(h * 128, (h + 1) * 128), 256 + np.arange(h * 2048, (h + 1) * 2048)]) for h in range(2)]
    for l in range(2):
        m = mod[l]
        mw = lambda b, j: m[b, j * D:(j + 1) * D]
        ims = []
        xins = []
        for core in range(8):
            b, h = core // 2, core % 2
            xin = A(np.concatenate([ctx[b, h * 128:(h + 1) * 128], x[b, h * 2048:(h + 1) * 2048]], 0))
            xins.append(xin)
            sc = np.concatenate([_colT(mw(4, 1)), _colT(mw(b, 1))], 1)
            sh = np.concatenate([_colT(mw(4, 0)), _colT(mw(b, 0))], 1)
            ims.append({"xin": xin, "W": A(w_in[l]), "sc": A(sc), "sh": A(sh), "g": _colT(A(norm1_g[l])), "ident": ident})
        res = _run(nc1, ims)
        P = np.empty((4, T, 11680), f32)
        for core in range(8):
            b, h = core // 2, core % 2
            P[b, toks[h]] = res[core]["P"]
        del res
        cw9 = A(conv_w[l]).reshape(9, 3840)
        ims = []
        for core in range(8):
            pj = np.empty((15 * 128, T), f32)
            cwm = np.empty((128, 15 * 9), f32)
            for i in range(15):
                b, tl = divmod(core * 15 + i, 30)
                pj[i * 128:(i + 1) * 128] = P[b, :, tl * 128:(tl + 1) * 128].T
                cwm[:, i * 9:(i + 1) * 9] = cw9[:, tl * 128:(tl + 1) * 128].T
            ims.append({"PJ": pj, "CW": cwm})
        res = _run(nc2a, ims)
        CV = np.empty((4, 3840, T), f32)
        for core in range(8):
            for i in range(15):
                b, tl = divmod(core * 15 + i, 30)
                CV[b, tl * 128:(tl + 1) * 128] = res[core]["CV"][i * 128:(i + 1) * 128]
        del res
        ims = []
        for core in range(8):
            b, s = core // 2, core % 2
            cs = slice(s * 384, (s + 1) * 384)
            r_fm, k_fm, v_fm = CV[b, 0:768][cs], CV[b, 768:1536][cs], CV[b, 1536:2304][cs]
            RK = np.stack([np.concatenate([r_fm[:, idx[d]].T, k_fm[:, idx[d]].T], 1) for d in range(2)])
            VS = np.stack([v_fm[:, idx[d]] for d in range(2)])
            ims.append({
                "RK": A(RK), "VFM": A(VS.reshape(2, 6, 64, T).transpose(0, 2, 1, 3).reshape(128, 6, T)),
                "VTOK": A(VS.transpose(0, 2, 1)),
                "LW": A(np.stack([P[b, idx[d], 3840 + d * 64:3840 + (d + 1) * 64].T for d in range(2)])),
                "LA": A(np.stack([P[b, idx[d], 3968 + d * 64:3968 + (d + 1) * 64].T for d in range(2)])),
                "LG": A(P[b, :, 4096:4224].T),
                "W2": A(np.concatenate([rk_w2[l][:, :, cs], rk_w0[l][:, None, cs]], 1)),
                "A2": A(np.concatenate([rk_a2[l][:, :, cs], rk_a0[l][:, None, cs]], 1)),
                "G2": A(rk_g2[l][:, cs]),
                "BCS": A(np.concatenate([_bc(A(v[l])[cs]) for v in (rk_kk, rk_ka, rk_rk, rk_gn_w, rk_gn_b)], 1)),
                "SEL": SEL, "I2": I2})
        res = _run(ncR, ims)
        ZA = np.empty((4, 2, T, 768), f32)
        GA = np.empty((4, T, 768), f32)
        for core in range(8):
            b, s = core // 2, core % 2
            for d in range(2):
                ZA[b, d, :, s * 384:(s + 1) * 384] = res[core]["Z"][d][idx[d]]
            GA[b, :, s * 384:(s + 1) * 384] = res[core]["G"]
        del res
        ims = []
        for core in range(8):
            b, s = core // 2, core % 2
            QKF = np.empty((4, 2, 96, T), f32)
            KT = np.empty((4, T, 96), f32)
            VT = np.empty((4, T, 192), f32)
            for u in range(4):
                d, hh = u // 2, u % 2
                h = 2 * s + hh
                q_fm = CV[b, 2304 + h * 96:2304 + (h + 1) * 96][:, idx[d]]
                k_fm = CV[b, 2688 + h * 96:2688 + (h + 1) * 96][:, idx[d]]
                v_fm = CV[b, 3072 + h * 192:3072 + (h + 1) * 192][:, idx[d]]
                QKF[u, 0], QKF[u, 1] = q_fm, k_fm
                KT[u] = k_fm.T
                VT[u] = v_fm.T
            ks = slice(s * 192, (s + 1) * 192)
            ims.append({"QKF": QKF, "KT": KT, "VT": VT,
                        "DA": A(np.stack([P[b, idx[d], 4224 + d * 16:4224 + (d + 1) * 16].T for d in range(2)])),
                        "A2": A(np.concatenate([gla_a2[l][:, :, ks], gla_ab[l][:, None, ks]], 1)), "TRI": TRI})
        res = _run(ncG, ims)
        OB = np.empty((4, 2, T, 768), f32)
        for core in range(8):
            b, s = core // 2, core % 2
            for d in range(2):
                OB[b, d, :, s * 384:(s + 1) * 384] = res[core]["O"][d][idx[d]]
        del res
        ims = []
        for core in range(8):
            b, s = core // 2, core % 2
            gsl = slice(16 * s, 16 * s + 16)
            UF = np.stack([P[b, idx[d], 5024 + s * 256:5024 + (s + 1) * 256].T for d in range(2)])
            LRI = np.concatenate([_s5_lay(A(s5_lam_re[l][:, gsl])), _s5_lay(A(s5_lam_im[l][:, gsl])),
                                  _s5_lay(A(np.broadcast_to(s5_log_dt[l][:, gsl, None], (2, 16, 64))))], 1)
            BT = np.zeros((32, 2, 8, 128), f32)
            CT = np.zeros((128, 2, 8, 32), f32)
            for j in range(8):
                for gg in range(2):
                    g = 16 * s + 2 * j + gg
                    BT[gg * 16:(gg + 1) * 16, 0, j, gg * 64:(gg + 1) * 64] = s5_b_re[l][g].T
                    BT[gg * 16:(gg + 1) * 16, 1, j, gg * 64:(gg + 1) * 64] = s5_b_im[l][g].T
                    CT[gg * 64:(gg + 1) * 64, 0, j, gg * 16:(gg + 1) * 16] = s5_c_re[l][g].T
                    CT[gg * 64:(gg + 1) * 64, 1, j, gg * 16:(gg + 1) * 16] = s5_c_im[l][g].T
            ims.append({"UF": A(UF), "LRI": A(LRI), "BT": BT.reshape(32, -1), "CT": CT.reshape(128, -1)})
        res = _run(ncS, ims)
        YCF = np.empty((4, 2, 512, T), f32)
        for core in range(8):
            b, s = core // 2, core % 2
            for d in range(2):
                YCF[b, d, s * 256:(s + 1) * 256] = res[core]["YC"][d][:, idx[d]]
        del res
        ims = []
        for core in range(8):
            b, h = core // 2, core % 2
            tk = toks[h]
            MODB = np.stack([np.stack([_bc(mw(w, 2)), _bc(mw(w, 4)), _bc(mw(w, 3))]) for w in (4, b)])
            ims.append({"XIN": xins[core], "ZA": A(ZA[b][:, tk]), "GA": A(GA[b][tk]), "OB": A(OB[b][:, tk]),
                        "OG": A(P[b, tk, 4256:5024]), "YCF": A(YCF[b][:, :, tk]), "UF": A(P[b, tk, 5024:5536].T),
                        "BG": A(P[b, tk, 5536:]), "MODB": A(MODB), "G2B": _bc(A(norm2_g[l])),
                        "NGB": _bc(np.tile(A(gla_norm_g[l]), 4)), "DSK": _colT(A(s5_d[l]), 4),
                        "PA": A(proj_a[l]), "PB": A(proj_b[l]), "PC": A(proj_c[l]), "WO": A(w_out[l]),
                        "GLU": A(s5_glu_w[l]), "ident": ident})
        res = _run(nc3a, ims)
        del P, CV, ZA, GA, OB, YCF
        last = (l == 1)
        ims2 = []
        for core in range(8):
            b, h = core // 2, core % 2
            ims2.append({"H2T": A(res[core]["H2"].T), "XMID": res[core]["XMID"],
                         "GA2B": A(np.stack([_bc(mw(4, 5)), _bc(mw(b, 5))])), "RW": A(router_w),
                         "RBB": _bc(A(router_bias)), "W1": A(exp_w1[l]), "W3": A(exp_w3[l]), "W2": A(exp_w2[l]),
                         "FGB": _bc(A(final_g))})
        del res
        res = _run(build_L3b(17, last), ims2)
        for core in range(8):
            b, h = core // 2, core % 2
            o = res[core]["OUT"]
            ctx[b, h * 128:(h + 1) * 128] = o[0:128]
            x[b, h * 2048:(h + 1) * 2048] = o[128:]
        del res
    return x
```

```python
from concourse.bass_utils import run_bass_kernel_spmd
import numpy as np
from contextlib import ExitStack
import concourse.bass as bass
import concourse.mybir as mybir

F32 = mybir.dt.float32
BF16 = mybir.dt.bfloat16
ALU = mybir.AluOpType
AF = mybir.ActivationFunctionType
AX = mybir.AxisListType


class KB:
    def __init__(self, nc, n_dma_sems=40):
        self.nc = nc
        self.es = ExitStack()
        self.engs = {'pe': nc.tensor, 'dve': nc.vector, 'act': nc.scalar, 'pool': nc.gpsimd, 'sp': nc.sync}
        self.sems = {}
        self.cnt = {}
        for e in self.engs:
            self.sems[e] = self.es.enter_context(nc.semaphore('s_' + e))
            self.cnt[e] = 0
        self.ndma = n_dma_sems
        for i in range(n_dma_sems):
            k = 'd%d' % i
            self.sems[k] = self.es.enter_context(nc.semaphore('s_' + k))
            self.cnt[k] = 0
        self.dma_rr = 0
        self.known = {e: {} for e in self.engs}
        self.acc = {}
        self.rows = {}
        self.ninst = 0
        self.psum_names = set()

    def sbuf(self, name, shape, dt=F32):
        t = self.es.enter_context(self.nc.sbuf_tensor(name, list(shape), dt))
        self.rows[name] = int(np.prod(shape[1:]))
        return t

    def psum(self, name, shape, dt=F32):
        t = self.es.enter_context(self.nc.psum_tensor(name, list(shape), dt))
        self.rows[name] = int(np.prod(shape[1:]))
        self.psum_names.add(name)
        return t

    def dram(self, name, shape, dt=F32, kind="Internal"):
        t = self.nc.dram_tensor(name, list(shape), dt, kind=kind)
        return t

    def track_dram(self, name):
        self.rows[name] = None

    def _bbox(self, ap):
        name = ap.tensor.name
        if name not in self.rows:
            return None
        row = self.rows[name]
        if row is None or name in self.psum_names:
            return (name, 0, 1 << 30, 0, 1 << 30)
        a = ap.ap
        off = ap.offset
        p0 = off // row
        f0 = off % row
        if a[0][0] == row:
            p1 = p0 + a[0][1]
            rest = a[1:]
        elif a[0][0] == 0 and len(a) > 1:
            p1 = p0 + 1
            rest = a[1:]
        else:
            p1 = p0 + 1
            rest = a
        ext = 0
        for st, c in rest:
            ext += abs(st) * (c - 1)
        return (name, p0, p1, f0, f0 + ext + 1)

    def _deps(self, e, outs, ins):
        deps = {}

        def need(sk, v):
            if deps.get(sk, 0) < v:
                deps[sk] = v

        boxes_in = [b for b in (self._bbox(a) for a in ins) if b is not None]
        boxes_out = [b for b in (self._bbox(a) for a in outs) if b is not None]
        for (name, p0, p1, f0, f1) in boxes_in:
            ps_ = name in self.psum_names
            for ent in self.acc.get(name, ()):
                if (ent[4] or (ps_ and ent[5] != e)) and ent[0] < p1 and p0 < ent[1] and ent[2] < f1 and f0 < ent[3]:
                    need(ent[5], ent[6])
        for (name, p0, p1, f0, f1) in boxes_out:
            for ent in self.acc.get(name, ()):
                if ent[0] < p1 and p0 < ent[1] and ent[2] < f1 and f0 < ent[3]:
                    if e == 'pe' and ent[5] == 'pe' and ent[4]:
                        continue
                    need(ent[5], ent[6])
        return deps, boxes_in, boxes_out

    def _record(self, sk, v, boxes_in, boxes_out):
        for (name, p0, p1, f0, f1) in boxes_out:
            lst = self.acc.setdefault(name, [])
            lst[:] = [en for en in lst if not (p0 <= en[0] and en[1] <= p1 and f0 <= en[2] and en[3] <= f1)]
            lst.append((p0, p1, f0, f1, True, sk, v))
        for (name, p0, p1, f0, f1) in boxes_in:
            lst = self.acc.setdefault(name, [])
            lst[:] = [en for en in lst if not ((not en[4]) and en[5] == sk and p0 <= en[0] and en[1] <= p1 and f0 <= en[2] and en[3] <= f1)]
            lst.append((p0, p1, f0, f1, False, sk, v))

    def _waits(self, e, deps):
        eng = self.engs[e]
        kn = self.known[e]
        for sk, v in deps.items():
            if kn.get(sk, 0) < v:
                eng.wait_ge(self.sems[sk], v)
                kn[sk] = v

    def op(self, e, fn, outs=(), ins=()):
        deps, bi, bo = self._deps(e, outs, ins)
        self._waits(e, deps)
        inst = fn(self.engs[e])
        self.cnt[e] += 1
        inst.then_inc(self.sems[e], 1)
        self._record(e, self.cnt[e], bi, bo)
        self.ninst += 1
        return inst

    def dma(self, e, out, in_, sem=None, **kw):
        if sem is None:
            sem = 'd%d' % self.dma_rr
            self.dma_rr = (self.dma_rr + 1) % self.ndma
        deps, bi, bo = self._deps(e, [out], [in_])
        if self.cnt[sem] > deps.get(sem, 0):
            deps[sem] = self.cnt[sem]
        self._waits(e, deps)
        inst = self.engs[e].dma_start(out=out, in_=in_, **kw)
        self.cnt[sem] += 16
        inst.then_inc(self.sems[sem], 16)
        self._record(sem, self.cnt[sem], bi, bo)
        self.ninst += 1
        return inst

    def finish(self, e='sp'):
        deps = {}
        for sk, v in self.cnt.items():
            if v > 0:
                deps[sk] = v
        self._waits(e, deps)

    def close(self):
        self.es.close()

    def mm(self, out, lhsT, rhs, start=True, stop=True, **kw):
        return self.op('pe', lambda g: g.matmul(out, lhsT, rhs, start=start, stop=stop, **kw), [out], [lhsT, rhs])

    def tr(self, out, in_, ident):
        return self.op('pe', lambda g: g.transpose(out, in_, ident), [out], [in_, ident])

    def tt(self, e, out, a, b, op_):
        return self.op(e, lambda g: g.tensor_tensor(out, a, b, op_), [out], [a, b])

    def ts(self, e, out, a, s1, s2, op0, op1=None):
        ins = [a] + [s for s in (s1, s2) if isinstance(s, bass.AP)]
        if op1 is None:
            return self.op(e, lambda g: g.tensor_scalar(out, a, s1, None, op0), [out], ins)
        return self.op(e, lambda g: g.tensor_scalar(out, a, s1, s2, op0, op1), [out], ins)

    def stt(self, out, a, s, b, op0, op1, e='dve'):
        ins = [a, b] + ([s] if isinstance(s, bass.AP) else [])
        return self.op(e, lambda g: g.scalar_tensor_tensor(out, a, s, b, op0, op1), [out], ins)

    def actf(self, out, a, func, bias=None, scale=None, accum_out=None):
        kw = {}
        ins = [a]
        outs = [out]
        if bias is not None:
            kw['bias'] = bias
            if isinstance(bias, bass.AP):
                ins.append(bias)
        if scale is not None:
            kw['scale'] = scale
            if isinstance(scale, bass.AP):
                ins.append(scale)
        if accum_out is not None:
            kw['accum_out'] = accum_out
            outs.append(accum_out)
        return self.op('act', lambda g: g.activation(out, a, func, **kw), outs, ins)

    def copy(self, e, out, a):
        if e == 'act':
            return self.op(e, lambda g: g.copy(out, a), [out], [a])
        return self.op(e, lambda g: g.tensor_copy(out, a), [out], [a])

    def memset(self, e, out, val):
        return self.op(e, lambda g: g.memset(out, val), [out], [])

    def red(self, out, a, op_=None, axis=None, e='dve'):
        op_ = op_ or ALU.add
        axis = axis or AX.X
        return self.op(e, lambda g: g.tensor_reduce(out, a, axis, op_), [out], [a])


D = 2048
NCORE = 8


def _run(nc, in_maps):
    res = run_bass_kernel_spmd(nc, in_maps, core_ids=list(range(NCORE)))
    return res.results


def _colT(v, n=16):
    return np.ascontiguousarray(v.reshape(n, 128).T)


def _bc(v, p=128):
    return np.ascontiguousarray(np.broadcast_to(v[None, :], (p, v.shape[0])))


def build_L0():
    nc = bass.Bass("TRN2", target_bir_lowering=False)
    vt = nc.dram_tensor("vt", [128, 80], F32, kind="ExternalInput").ap()
    W = nc.dram_tensor("W", [2048, 3072], F32, kind="ExternalInput").ap()
    bias = nc.dram_tensor("bias", [5, 3072], F32, kind="ExternalInput").ap()
    M = nc.dram_tensor("M", [5, 3072], F32, kind="ExternalOutput").ap()
    kb = KB(nc)
    v = kb.sbuf("v", [128, 80])
    sv = kb.sbuf("sv", [128, 80])
    bs = kb.sbuf("bs", [5, 3072])
    o = kb.sbuf("o", [5, 3072])
    wt = [kb.sbuf("wt%d" % i, [128, 16, 512]) for i in range(2)]
    ps = [kb.psum("ps%d" % i, [128, 512]) for i in range(2)]
    kb.dma('sp', v[:], vt)
    kb.dma('sp', bs[:], bias)
    kb.actf(sv[:], v[:], AF.Silu)
    Wv = W.rearrange("(k p) n -> p k n", p=128)
    for n in range(6):
        w = wt[n % 2]
        kb.dma('sp' if n % 2 == 0 else 'act', w[:], Wv[:, :, n * 512:(n + 1) * 512])
        p = ps[n % 2]
        for k in range(16):
            kb.mm(p[0:5, :], sv[:, k * 5:(k + 1) * 5], w[:, k, :], start=(k == 0), stop=(k == 15))
        kb.tt('dve', o[:, n * 512:(n + 1) * 512], p[0:5, :], bs[:, n * 512:(n + 1) * 512], ALU.add)
    kb.dma('sp', M, o[:])
    kb.finish('sp')
    kb.close()
    return nc


def _rms_rstd(kb, rstd, ssq, n, eps):
    kb.ts('dve', rstd, ssq, 1.0 / n, eps, ALU.mult, ALU.add)
    kb.actf(rstd, rstd, AF.Sqrt)
    kb.op('dve', lambda g: g.reciprocal(rstd, rstd), [rstd], [rstd])


def build_L1(NT=17, NCOL=11680):
    nc = bass.Bass("TRN2", target_bir_lowering=False)
    xin = nc.dram_tensor("xin", [NT * 128, D], F32, kind="ExternalInput").ap()
    W = nc.dram_tensor("W", [D, NCOL], F32, kind="ExternalInput").ap()
    sc = nc.dram_tensor("sc", [128, 32], F32, kind="ExternalInput").ap()
    sh = nc.dram_tensor("sh", [128, 32], F32, kind="ExternalInput").ap()
    g = nc.dram_tensor("g", [128, 16], F32, kind="ExternalInput").ap()
    ident = nc.dram_tensor("ident", [128, 128], F32, kind="ExternalInput").ap()
    P = nc.dram_tensor("P", [NT * 128, NCOL], F32, kind="ExternalOutput").ap()
    kb = KB(nc)
    hT = kb.sbuf("hT", [128, 16, NT * 128], BF16)
    idt = kb.sbuf("idt", [128, 128])
    scs = kb.sbuf("scs", [128, 32])
    shs = kb.sbuf("shs", [128, 32])
    gs = kb.sbuf("gs", [128, 16])
    eff = kb.sbuf("eff", [128, 32])
    xt = [kb.sbuf("xt%d" % i, [128, D]) for i in range(2)]
    junk = kb.sbuf("junk", [128, D])
    ssq = kb.sbuf("ssq", [128, 2])
    rstd = kb.sbuf("rstd", [128, 2])
    ps = [kb.psum("ps%d" % i, [128, 512]) for i in range(8)]
    kb.dma('sp', idt[:], ident)
    kb.dma('sp', scs[:], sc)
    kb.dma('sp', shs[:], sh)
    kb.dma('sp', gs[:], g)
    for w in range(2):
        kb.stt(eff[:, w * 16:(w + 1) * 16], scs[:, w * 16:(w + 1) * 16], 1.0, gs[:], ALU.add, ALU.mult)
    for t in range(NT):
        x_ = xt[t % 2]
        c = t % 2
        kb.dma('sp', x_[:], xin[t * 128:(t + 1) * 128, :])
        kb.actf(junk[:], x_[:], AF.Square)
        kb.red(ssq[:, c:c + 1], junk[:])
        _rms_rstd(kb, rstd[:, c:c + 1], ssq[:, c:c + 1], D, 1e-6)
        kb.ts('dve', x_[:], x_[:], rstd[:, c:c + 1], None, ALU.mult)
        w = 0 if t == 0 else 1
        for j in range(16):
            p = ps[(j // 4) % 2]
            sl = p[:, (j % 4) * 128:(j % 4 + 1) * 128]
            kb.tr(sl, x_[:, j * 128:(j + 1) * 128], idt[:])
            kb.ts('dve', hT[:, j, t * 128:(t + 1) * 128], sl, eff[:, w * 16 + j:w * 16 + j + 1],
                  shs[:, w * 16 + j:w * 16 + j + 1], ALU.mult, ALU.add)
    nch = (NCOL + 511) // 512
    wb = [kb.sbuf("wb%d" % i, [128, 16, 512], BF16) for i in range(2)]
    ot = [kb.sbuf("ot%d" % i, [128, 512]) for i in range(4)]
    Wv = W.rearrange("(k p) n -> p k n", p=128)
    cnt = 0
    for c in range(nch):
        c0 = c * 512
        cw = min(512, NCOL - c0)
        w = wb[c % 2]
        kb.dma('pool', w[:, :, 0:cw], Wv[:, :, c0:c0 + cw])
        for t in range(NT):
            p = ps[2 + cnt % 6]
            for k in range(16):
                kb.mm(p[:, 0:cw], hT[:, k, t * 128:(t + 1) * 128], w[:, k, 0:cw], start=(k == 0), stop=(k == 15))
            o = ot[cnt % 4]
            kb.copy('act' if cnt % 2 == 0 else 'dve', o[:, 0:cw], p[:, 0:cw])
            kb.dma('sp' if cnt % 2 == 0 else 'act', P[t * 128:(t + 1) * 128, c0:c0 + cw], o[:, 0:cw])
            cnt += 1
    kb.finish('sp')
    kb.close()
    return nc


T_ALL = 4352
_GS = 6
_GDBG = False
NCH = 68


def build_L2a(NU=15):
    nc = bass.Bass("TRN2", target_bir_lowering=False)
    PJ = nc.dram_tensor("PJ", [NU * 128, T_ALL], F32, kind="ExternalInput").ap()
    CW = nc.dram_tensor("CW", [128, NU * 9], F32, kind="ExternalInput").ap()
    CV = nc.dram_tensor("CV", [NU * 128, T_ALL], F32, kind="ExternalOutput").ap()
    kb = KB(nc)
    cwt = kb.sbuf("cwt", [128, NU * 9])
    xi = [kb.sbuf("xi%d" % i, [128, T_ALL]) for i in range(2)]
    xo = [kb.sbuf("xo%d" % i, [128, T_ALL]) for i in range(2)]
    kb.dma('sp', cwt[:], CW)
    for u in range(NU):
        a = xi[u % 2]
        o = xo[u % 2]
        kb.dma('sp', a[:], PJ[u * 128:(u + 1) * 128, :])
        wv = lambda tap: cwt[:, u * 9 + tap:u * 9 + tap + 1]
        kb.ts('dve', o[:], a[:], wv(4), None, ALU.mult)
        for j in (0, 2):
            dx = j - 1
            c0, c1 = max(0, -dx), 256 - max(0, dx)
            kb.stt(o[:, c0:c1], a[:, c0 + dx:c1 + dx], wv(3 + j), o[:, c0:c1], ALU.mult, ALU.add)
        li = a[:, 256:].rearrange("p (r c) -> p r c", c=64)
        lo = o[:, 256:].rearrange("p (r c) -> p r c", c=64)
        for i in range(3):
            for j in range(3):
                if i == 1 and j == 1:
                    continue
                dy, dx = i - 1, j - 1
                r0, r1 = max(0, -dy), 64 - max(0, dy)
                c0, c1 = max(0, -dx), 64 - max(0, dx)
                kb.stt(lo[:, r0:r1, c0:c1], li[:, r0 + dy:r1 + dy, c0 + dx:c1 + dx], wv(i * 3 + j),
                       lo[:, r0:r1, c0:c1], ALU.mult, ALU.add)
        kb.dma('act', CV[u * 128:(u + 1) * 128, :], o[:])
    kb.finish('sp')
    kb.close()
    return nc


A_DECAY_SCALE = float(np.exp(-0.5))


def build_LR(NCHUNK=NCH):
    T = NCHUNK * 64
    nc = bass.Bass("TRN2", target_bir_lowering=False)
    din = lambda n, s: nc.dram_tensor(n, s, F32, kind="ExternalInput").ap()
    RK = din("RK", [2, T, 768])
    VFM = din("VFM", [128, 6, T])
    VTOK = din("VTOK", [2, T, 384])
    LW = din("LW", [2, 64, T])
    LA = din("LA", [2, 64, T])
    LG = din("LG", [128, T])
    W2 = din("W2", [2, 65, 384])
    A2 = din("A2", [2, 65, 384])
    G2 = din("G2", [128, 384])
    BCS = din("BCS", [128, 5 * 384])
    SEL = din("SEL", [128, 64 * 128])
    I2 = din("I2", [128, 64])
    Z = nc.dram_tensor("Z", [2, T, 384], F32, kind="ExternalOutput").ap()
    G = nc.dram_tensor("G", [T, 384], F32, kind="ExternalOutput").ap()
    DQ = nc.dram_tensor("DQ", [2, T, 1920], F32, kind="Internal").ap()
    kb = KB(nc)
    kb.track_dram("DQ")
    bcs = kb.sbuf("bcs", [128, 5, 6, 64])
    sel = kb.sbuf("sel", [128, 64 * 128])
    i2 = kb.sbuf("i2", [128, 64])
    w2 = kb.sbuf("w2", [65, 2, 384])
    a2 = kb.sbuf("a2", [65, 2, 384])
    g2 = kb.sbuf("g2", [128, 384])
    kb.dma('sp', bcs[:].rearrange("p a h k -> p (a h k)"), BCS)
    kb.dma('sp', sel[:], SEL)
    kb.dma('sp', i2[:], I2)
    kb.dma('sp', g2[:], G2)
    for d in range(2):
        kb.dma('sp', w2[:, d, :], W2[d])
        kb.dma('sp', a2[:, d, :], A2[d])
    kkbc, kabc, rkbc, gnw, gnb = [bcs[:, i] for i in range(5)]
    ps = [kb.psum("ps%d" % i, [128, 512]) for i in range(8)]
    lw = [kb.sbuf("lw%d" % i, [65, 128]) for i in range(2)]
    la = [kb.sbuf("la%d" % i, [65, 128]) for i in range(2)]
    lg = [kb.sbuf("lg%d" % i, [128, 128]) for i in range(2)]
    dq = [kb.sbuf("dq%d" % i, [128, 5, 6, 64]) for i in range(2)]
    kt = [kb.sbuf("kt%d" % i, [128, 6, 64]) for i in range(2)]
    at = kb.sbuf("at", [128, 6, 64])
    t1 = kb.sbuf("t1", [128, 6, 64])
    t2 = kb.sbuf("t2", [128, 6, 64])
    s6 = kb.sbuf("s6", [128, 8])
    gt = [kb.sbuf("gt%d" % i, [128, 384]) for i in range(2)]
    for i in range(2):
        kb.memset('dve', lw[i][:], 1.0)
        kb.memset('dve', la[i][:], 1.0)
    f3 = lambda ap: ap.rearrange("p h k -> p (h k)")
    bc6 = lambda ap: ap.unsqueeze(2).broadcast_to([128, 6, 64])
    it = 0
    for d in range(2):
        for t in range(T // 128):
            b = it % 2
            it += 1
            ts_ = slice(t * 128, (t + 1) * 128)
            q = dq[b]
            k_ = kt[b]
            kb.dma('sp', f3(q[:, 4]), RK[d, ts_, 0:384])
            kb.dma('sp', f3(k_[:]), RK[d, ts_, 384:768])
            kb.dma('act', lw[b][0:64, :], LW[d, :, ts_])
            kb.dma('act', la[b][0:64, :], LA[d, :, ts_])
            kb.actf(lw[b][0:64, :], lw[b][0:64, :], AF.Tanh)
            pw = ps[0]
            pa = ps[1]
            kb.mm(pw[:, 0:384], lw[b][:, :], w2[:, d, :])
            kb.mm(pa[:, 0:384], la[b][:, :], a2[:, d, :])
            kb.actf(f3(t1[:]), pw[:, 0:384], AF.Sigmoid)
            kb.actf(f3(q[:, 1]), f3(t1[:]), AF.Exp, scale=-A_DECAY_SCALE)
            kb.actf(f3(at[:]), pa[:, 0:384], AF.Sigmoid)
            kb.tt('dve', t1[:], k_[:], kkbc, ALU.mult)
            kb.tt('dve', t2[:], t1[:], t1[:], ALU.mult)
            kb.red(s6[:, 0:6], t2[:])
            kb.ts('dve', s6[:, 0:6], s6[:, 0:6], 1e-12, None, ALU.add)
            kb.actf(s6[:, 0:6], s6[:, 0:6], AF.Sqrt)
            kb.op('dve', lambda g: g.reciprocal(s6[:, 0:6], s6[:, 0:6]), [s6[:, 0:6]], [s6[:, 0:6]])
            kb.tt('dve', q[:, 0], t1[:], bc6(s6[:, 0:6]), ALU.mult)
            kb.tt('dve', q[:, 2], q[:, 0], at[:], ALU.mult)
            kb.stt(t2[:], at[:], -1.0, kabc, ALU.add, ALU.mult)
            kb.stt(q[:, 3], t2[:], 1.0, k_[:], ALU.add, ALU.mult)
            kb.dma('sp', DQ[d, ts_, :], q[:].rearrange("p a h k -> p (a h k)"))
            if d == 0:
                kb.dma('act', lg[b][:], LG[:, ts_])
                kb.actf(lg[b][:], lg[b][:], AF.Sigmoid)
                pg = ps[2]
                kb.mm(pg[:, 0:384], lg[b][:], g2[:])
                kb.copy('act', gt[b][:], pg[:, 0:384])
                kb.dma('sp', G[ts_, :], gt[b][:])
    S = kb.sbuf("S", [128, 6, 64])
    tmp = kb.sbuf("tmp", [128, 6, 64])
    tmp2 = kb.sbuf("tmp2", [128, 6, 64])
    sa = kb.sbuf("sa", [128, 6])
    pt = [kb.sbuf("pt%d" % i, [128, 5, 6, 64]) for i in range(2)]
    vv = [kb.sbuf("vv%d" % i, [128, 6, 64]) for i in range(2)]
    vtk = [kb.sbuf("vtk%d" % i, [128, 6, 64]) for i in range(2)]
    ycol = [kb.sbuf("ycol%d" % i, [128, 6, 64]) for i in range(2)]
    yblk = [kb.sbuf("yblk%d" % i, [128, 6, 128]) for i in range(2)]
    yt = kb.sbuf("yt", [128, 6, 64])
    cen = kb.sbuf("cen", [128, 6, 64])
    zt = [kb.sbuf("zt%d" % i, [128, 6, 64]) for i in range(2)]
    st = kb.sbuf("st", [128, 24])
    for i in range(2):
        kb.memset('dve', yblk[i][:], 0.0)
    kb.memset('dve', S[:], 0.0)
    pi = 0
    for c in range(NCHUNK):
        b = c % 2
        cs = slice(c * 64, (c + 1) * 64)
        p_ = pt[b]
        pflat = p_[:].rearrange("p a h k -> p (a h k)")
        kb.dma('sp', pflat[0:64, :], DQ[0, cs, :])
        kb.dma('act', pflat[64:128, :], DQ[1, cs, :])
        kb.dma('sp', vv[b][:], VFM[:, :, cs])
        kb.dma('act', f3(vtk[b][0:64]), VTOK[0, cs, :])
        kb.dma('act', f3(vtk[b][64:128]), VTOK[1, cs, :])
        yc = ycol[b]
        for t in range(64):
            lhsT = sel[:, t * 128:(t + 1) * 128]
            bcl = []
            for j in range(5):
                p = ps[3 + pi % 5]
                pi += 1
                kb.mm(p[:, 0:384], lhsT, f3(p_[:, j]))
                bcl.append(p[:, 0:384].rearrange("p (h k) -> p h k", h=6))
            kkb, wb, kab, kdb, rb = bcl
            kb.tt('dve', tmp[:], S[:], kkb, ALU.mult)
            kb.red(sa[:], tmp[:])
            kb.tt('dve', S[:], S[:], wb, ALU.mult)
            kb.tt('dve', tmp2[:], kab, bc6(sa[:]), ALU.mult)
            kb.tt('dve', S[:], S[:], tmp2[:], ALU.subtract)
            kb.tt('dve', tmp2[:], kdb, bc6(vv[b][:, :, t]), ALU.mult)
            kb.tt('dve', S[:], S[:], tmp2[:], ALU.add)
            kb.tt('dve', tmp[:], S[:], rb, ALU.mult)
            kb.red(yc[:, :, t], tmp[:])
        yb_ = yblk[b]
        kb.copy('act', yb_[0:64, :, 0:64], yc[0:64])
        kb.copy('act', yb_[64:128, :, 64:128], yc[64:128])
        py = ps[0]
        for h in range(6):
            kb.mm(py[:, h * 64:(h + 1) * 64], yb_[:, h, :], i2[:])
        kb.copy('act', f3(yt[:]), py[:, 0:384])
        kb.red(st[:, 0:6], yt[:])
        kb.ts('dve', st[:, 0:6], st[:, 0:6], -1.0 / 64, None, ALU.mult)
        kb.tt('dve', cen[:], yt[:], bc6(st[:, 0:6]), ALU.add)
        kb.tt('dve', yt[:], cen[:], cen[:], ALU.mult)
        kb.red(st[:, 6:12], yt[:])
        _rms_rstd(kb, st[:, 6:12], st[:, 6:12], 64, 64e-5)
        kb.tt('dve', cen[:], cen[:], bc6(st[:, 6:12]), ALU.mult)
        kb.tt('dve', cen[:], cen[:], gnw, ALU.mult)
        kb.tt('dve', cen[:], cen[:], gnb, ALU.add)
        kb.tt('dve', yt[:], p_[:, 4], p_[:, 3], ALU.mult)
        kb.tt('dve', yt[:], yt[:], rkbc, ALU.mult)
        kb.red(st[:, 12:18], yt[:])
        z = zt[b]
        kb.tt('dve', z[:], vtk[b][:], bc6(st[:, 12:18]), ALU.mult)
        kb.tt('dve', z[:], z[:], cen[:], ALU.add)
        kb.dma('sp', Z[0, cs, :], f3(z[0:64]))
        kb.dma('act', Z[1, cs, :], f3(z[64:128]))
    kb.finish('sp')
    kb.close()
    return nc


def build_LG(NCHUNK=NCH):
    T = NCHUNK * 64
    nc = bass.Bass("TRN2", target_bir_lowering=False)
    din = lambda n, s: nc.dram_tensor(n, s, F32, kind="ExternalInput").ap()
    QKF = din("QKF", [4, 2, 96, T])
    KT = din("KT", [4, T, 96])
    VT = din("VT", [4, T, 192])
    DA = din("DA", [2, 16, T])
    A2 = din("A2", [2, 17, 192])
    TRI = din("TRI", [64, 128])
    O = nc.dram_tensor("O", [2, T, 384], F32, kind="ExternalOutput").ap()
    kb = KB(nc)
    tri = kb.sbuf("tri", [64, 128])
    a2 = kb.sbuf("a2", [17, 2, 192])
    kb.dma('sp', tri[:], TRI)
    for d in range(2):
        kb.dma('sp', a2[:, d, :], A2[d])
    tinc = tri[:, 0:64]
    taft = tri[:, 64:128]
    ps = [kb.psum("ps%d" % i, [128, 512]) for i in range(8)]
    U = []
    for u in range(4):
        b = {}
        b['S'] = kb.sbuf("S%d" % u, [96, 192])
        kb.memset('dve', b['S'][:], 0.0)
        for nm, shp in [("da", [17, 64]), ("qk", [96, 2, 64]), ("kt", [64, 96]), ("vt", [64, 192]), ("g", [64, 96]),
                        ("e1", [64, 96]), ("kdec", [64, 96]), ("cg", [96, 64]), ("sc", [96, 4]), ("eq", [96, 64]),
                        ("qp", [96, 64]), ("kp", [96, 64]), ("qd", [96, 64]), ("scm", [64, 64]), ("o", [64, 192])]:
            b[nm] = kb.sbuf("%s%d" % (nm, u), shp)
        kb.memset('dve', b['da'][:], 1.0)
        U.append(b)
    scale = 96.0 ** -0.5
    for c in range(NCHUNK):
        cs = slice(c * 64, (c + 1) * 64)
        for u in range(4):
            d, hh = u // 2, u % 2
            b = U[u]
            p0 = ps[(2 * u) % 8]
            p1 = ps[(2 * u + 1) % 8]
            kb.dma('sp', b['da'][0:16, :], DA[d, :, cs])
            kb.dma('sp', b['qk'][:], QKF[u, :, :, cs].rearrange("a k t -> k a t"))
            kb.dma('act', b['kt'][:], KT[u, cs, :])
            kb.dma('act', b['vt'][:], VT[u, cs, :])
            kb.mm(p0[0:64, 0:96], b['da'][:, :], a2[:, d, hh * 96:(hh + 1) * 96])
            kb.actf(b['g'][:], p0[0:64, 0:96], AF.Exp, scale=-1.0)
            kb.actf(b['g'][:], b['g'][:], AF.Ln, bias=1.0)
            kb.ts('dve', b['g'][:], b['g'][:], -1.0 / 16.0, None, ALU.mult)
            kb.mm(p0[0:64, 128:224], taft, b['g'][:])
            kb.mm(p0[0:96, 256:320], b['g'][:], tinc)
            kb.actf(b['e1'][:], p0[0:64, 128:224], AF.Exp)
            kb.tt('dve', b['kdec'][:], b['kt'][:], b['e1'][:], ALU.mult)
            kb.copy('act', b['cg'][:], p0[0:96, 256:320])
            sc = b['sc']
            kb.ts('dve', sc[:, 0:1], b['cg'][:, 31:32], -1.0, None, ALU.mult)
            kb.actf(sc[:, 1:2], b['cg'][:, 63:64], AF.Exp)
            kb.actf(b['eq'][:], b['cg'][:], AF.Exp, bias=sc[:, 0:1])
            kb.stt(b['qp'][:], b['eq'][:], scale, b['qk'][:, 0, :], ALU.mult, ALU.mult)
            kb.actf(b['eq'][:], b['cg'][:], AF.Exp, scale=-1.0, bias=b['cg'][:, 31:32])
            kb.tt('dve', b['kp'][:], b['eq'][:], b['qk'][:, 1, :], ALU.mult)
            kb.actf(b['eq'][:], b['cg'][:], AF.Exp)
            kb.stt(b['qd'][:], b['eq'][:], scale, b['qk'][:, 0, :], ALU.mult, ALU.mult)
            kb.mm(p1[0:96, 0:192], b['kdec'][:], b['vt'][:])
            kb.mm(p0[0:64, 384:448], b['kp'][:], b['qp'][:])
            kb.tt('dve', b['scm'][:], p0[0:64, 384:448], tinc, ALU.mult)
            kb.mm(p1[0:64, 256:448], b['qd'][:], b['S'][:], start=True, stop=False)
            kb.mm(p1[0:64, 256:448], b['scm'][:], b['vt'][:], start=False, stop=True)
            kb.copy('act', b['o'][:], p1[0:64, 256:448])
            kb.dma('sp', O[d, cs, hh * 192:(hh + 1) * 192], b['o'][:])
            kb.stt(b['S'][:], b['S'][:], sc[:, 1:2], p1[0:96, 0:192], ALU.mult, ALU.add)
    kb.finish('sp')
    kb.close()
    return nc


TWO_PI = float(2 * np.pi)


def _sin_reduced(kb, out, ang, tmp, tmpi, mask):
    kb.ts('dve', tmp, ang, 1.0 / TWO_PI, None, ALU.mult)
    kb.copy('dve', tmpi, tmp)
    kb.copy('dve', tmp, tmpi)
    kb.stt(out, tmp, -TWO_PI, ang, ALU.mult, ALU.add)
    kb.ts('dve', mask, out, float(np.pi), -TWO_PI, ALU.is_gt, ALU.mult)
    kb.tt('dve', out, out, mask, ALU.add)
    kb.ts('dve', mask, out, -float(np.pi), TWO_PI, ALU.is_lt, ALU.mult)
    kb.tt('dve', out, out, mask, ALU.add)
    kb.actf(out, out, AF.Sin)


def build_LS(NCK=17):
    CK = 256
    T = NCK * CK
    nc = bass.Bass("TRN2", target_bir_lowering=False)
    din = lambda n, s: nc.dram_tensor(n, s, F32, kind="ExternalInput").ap()
    UF = din("UF", [2, 256, T])
    LRI = din("LRI", [128, 48])
    BT = din("BT", [32, 2 * 8 * 128])
    CT = din("CT", [128, 2 * 8 * 32])
    YC = nc.dram_tensor("YC", [2, 256, T], F32, kind="ExternalOutput").ap()
    kb = KB(nc)
    lri = kb.sbuf("lri", [128, 48])
    bt = kb.sbuf("bt", [32, 2, 8, 128])
    ct = kb.sbuf("ct", [128, 2, 8, 32])
    kb.dma('sp', lri[:], LRI)
    kb.dma('sp', bt[:].rearrange("p a j m -> p (a j m)"), BT)
    kb.dma('sp', ct[:].rearrange("p a j m -> p (a j m)"), CT)
    kb.actf(ct[:, 1], ct[:, 1], AF.Copy, scale=-1.0)
    lr, li, ldt = lri[:, 0:16], lri[:, 16:32], lri[:, 32:48]
    W = kb.sbuf("W", [128, 16, 16])
    w = lambda i: W[:, i, :]
    wi = kb.sbuf("wi", [128, 16], mybir.dt.int32)
    dt, mag, ang, sn, cs_, tmp, msk, ar, ai, am1, rden, fr, fi = [w(i) for i in range(13)]
    kb.actf(dt, ldt, AF.Exp)
    kb.tt('dve', tmp, lr, dt, ALU.mult)
    kb.actf(mag, tmp, AF.Exp)
    kb.tt('dve', ang, li, dt, ALU.mult)
    _sin_reduced(kb, sn, ang, tmp, wi[:], msk)
    kb.ts('dve', ang, ang, float(np.pi / 2), None, ALU.add)
    _sin_reduced(kb, cs_, ang, tmp, wi[:], msk)
    kb.tt('dve', ar, mag, cs_, ALU.mult)
    kb.tt('dve', ai, mag, sn, ALU.mult)
    kb.ts('dve', am1, ar, -1.0, None, ALU.add)
    kb.tt('dve', tmp, lr, lr, ALU.mult)
    kb.tt('dve', msk, li, li, ALU.mult)
    kb.tt('dve', tmp, tmp, msk, ALU.add)
    kb.op('dve', lambda g: g.reciprocal(rden, tmp), [rden], [tmp])
    kb.tt('dve', fr, am1, lr, ALU.mult)
    kb.tt('dve', tmp, ai, li, ALU.mult)
    kb.tt('dve', fr, fr, tmp, ALU.add)
    kb.tt('dve', fr, fr, rden, ALU.mult)
    kb.tt('dve', fi, ai, lr, ALU.mult)
    kb.tt('dve', tmp, am1, li, ALU.mult)
    kb.tt('dve', fi, fi, tmp, ALU.subtract)
    kb.tt('dve', fi, fi, rden, ALU.mult)
    ps = [kb.psum("ps%d" % i, [128, 512]) for i in range(8)]
    uf = [kb.sbuf("uf%d" % i, [32, 2, 8, CK]) for i in range(2)]
    bu = [kb.sbuf("bu%d" % i, [128, 2, 16, CK]) for i in range(2)]
    X = [kb.sbuf("X%d" % i, [128, 2, 16, CK]) for i in range(2)]
    t4 = kb.sbuf("t4", [128, 4, 16])
    tm = kb.sbuf("tm", [128, 2, CK])
    yo = [kb.sbuf("yo%d" % i, [32, CK]) for i in range(4)]
    x0 = kb.sbuf("x0", [128, 2, 16])
    kb.memset('dve', x0[:], 0.0)
    pi = 0
    yi = 0
    for c in range(NCK):
        b = c % 2
        cs = slice(c * CK, (c + 1) * CK)
        for d in range(2):
            kb.dma('sp' if d == 0 else 'act', uf[b][:, d], UF[d, :, cs].rearrange("(j c) t -> c j t", c=32))
        for d in range(2):
            for j in range(8):
                col = d * 8 + j
                p = ps[pi % 4]
                pi += 1
                kb.mm(p[:, 0:CK], bt[:, 0, j, :], uf[b][:, d, j, :])
                kb.mm(p[:, CK:2 * CK], bt[:, 1, j, :], uf[b][:, d, j, :])
                pr, pim = p[:, 0:CK], p[:, CK:2 * CK]
                kb.ts('dve', tm[:, 0], pim, fi[:, col:col + 1], None, ALU.mult)
                kb.stt(bu[b][:, 0, col, :], pr, fr[:, col:col + 1], tm[:, 0], ALU.mult, ALU.subtract)
                kb.ts('dve', tm[:, 1], pr, fi[:, col:col + 1], None, ALU.mult)
                kb.stt(bu[b][:, 1, col, :], pim, fr[:, col:col + 1], tm[:, 1], ALU.mult, ALU.add)
        Xc = X[b]
        for t in range(CK):
            if t == 0:
                xr, xi = (x0[:, 0], x0[:, 1]) if c == 0 else (X[1 - b][:, 0, :, CK - 1], X[1 - b][:, 1, :, CK - 1])
            else:
                xr, xi = Xc[:, 0, :, t - 1], Xc[:, 1, :, t - 1]
            kb.tt('dve', t4[:, 0], ar, xr, ALU.mult)
            kb.tt('dve', t4[:, 1], ar, xi, ALU.mult)
            kb.tt('dve', t4[:, 2], ai, xi, ALU.mult)
            kb.tt('dve', t4[:, 3], ai, xr, ALU.mult)
            kb.tt('dve', t4[:, 0], t4[:, 0], t4[:, 2], ALU.subtract)
            kb.tt('dve', t4[:, 1], t4[:, 1], t4[:, 3], ALU.add)
            kb.tt('dve', Xc[:, 0, :, t], t4[:, 0], bu[b][:, 0, :, t], ALU.add)
            kb.tt('dve', Xc[:, 1, :, t], t4[:, 1], bu[b][:, 1, :, t], ALU.add)
        for d in range(2):
            for j in range(8):
                col = d * 8 + j
                p = ps[4 + pi % 4]
                pi += 1
                kb.mm(p[0:32, 0:CK], ct[:, 0, j, :], Xc[:, 0, col, :], start=True, stop=False)
                kb.mm(p[0:32, 0:CK], ct[:, 1, j, :], Xc[:, 1, col, :], start=False, stop=True)
                y = yo[yi % 4]
                yi += 1
                kb.copy('act', y[:], p[0:32, 0:CK])
                kb.dma('sp', YC[d, j * 32:(j + 1) * 32, cs], y[:])
    kb.finish('sp')
    kb.close()
    return nc


def build_L3a(NT=17):
    NTOK = NT * 128
    nc = bass.Bass("TRN2", target_bir_lowering=False)
    din = lambda n, s: nc.dram_tensor(n, s, F32, kind="ExternalInput").ap()
    XIN = din("XIN", [NTOK, D])
    ZA = din("ZA", [2, NTOK, 768])
    GA = din("GA", [NTOK, 768])
    OB = din("OB", [2, NTOK, 768])
    OG = din("OG", [NTOK, 768])
    YCF = din("YCF", [2, 512, NTOK])
    UF = din("UF", [512, NTOK])
    BG = din("BG", [NTOK, 6144])
    MODB = din("MODB", [2, 3, 128, D])
    G2B = din("G2B", [128, D])
    NGB = din("NGB", [128, 768])
    DSK = din("DSK", [128, 4])
    PA = din("PA", [768, D])
    PB = din("PB", [768, D])
    PC = din("PC", [512, D])
    WO = din("WO", [D, D])
    GLU = din("GLU", [512, 1024])
    ident = din("ident", [128, 128])
    XMID = nc.dram_tensor("XMID", [NTOK, D], F32, kind="ExternalOutput").ap()
    H2 = nc.dram_tensor("H2", [NTOK, D], F32, kind="ExternalOutput").ap()
    M = nc.dram_tensor("M", [NTOK, D], F32, kind="Internal").ap()
    kb = KB(nc)
    kb.track_dram("M")
    idt = kb.sbuf("idt", [128, 128])
    wbig = kb.sbuf("wbig", [128, 16 * D], BF16)
    gluw = kb.sbuf("gluw", [128, 4, 1024], BF16)
    modb = kb.sbuf("modb", [128, 3, D])
    g2b = kb.sbuf("g2b", [128, D])
    ngb = kb.sbuf("ngb", [128, 768])
    dsk = kb.sbuf("dsk", [128, 4])
    kb.dma('sp', idt[:], ident)
    kb.dma('sp', g2b[:], G2B)
    kb.dma('sp', ngb[:], NGB)
    kb.dma('sp', dsk[:], DSK)
    pa = wbig[:, 0:6 * D].rearrange("p (k n) -> p k n", k=6)
    pb = wbig[:, 6 * D:12 * D].rearrange("p (k n) -> p k n", k=6)
    pc = wbig[:, 12 * D:16 * D].rearrange("p (k n) -> p k n", k=4)
    kb.dma('pool', pa, PA.rearrange("(k p) n -> p k n", p=128))
    kb.dma('pool', pb, PB.rearrange("(k p) n -> p k n", p=128))
    kb.dma('pool', pc, PC.rearrange("(k p) n -> p k n", p=128))
    kb.dma('pool', gluw[:], GLU.rearrange("(k p) n -> p k n", p=128))
    ps = [kb.psum("ps%d" % i, [128, 512]) for i in range(8)]
    a0 = kb.sbuf("a0", [128, 768])
    a1 = kb.sbuf("a1", [128, 768])
    a2_ = kb.sbuf("a2_", [128, 768])
    yaT = kb.sbuf("yaT", [128, 6, 128], BF16)
    ybT = kb.sbuf("ybT", [128, 6, 128], BF16)
    ycT = kb.sbuf("ycT", [128, 4, 128], BF16)
    s8 = kb.sbuf("s8", [128, 8])
    yf = kb.sbuf("yf", [128, 3, 4, 128])
    gT = kb.sbuf("gT", [128, 4, 128], BF16)
    gt1 = kb.sbuf("gt1", [128, 4, 128])
    gt2 = kb.sbuf("gt2", [128, 4, 128])
    yc = kb.sbuf("yc", [128, 512])
    bg = kb.sbuf("bg", [128, 3, 512])
    m = kb.sbuf("m", [128, D])
    tq = kb.sbuf("tq", [128, 512])

    def transposes(dstT, src, n, pbase):
        for j in range(n):
            p = ps[pbase + (j // 4) % 2]
            sl = p[:, (j % 4) * 128:(j % 4 + 1) * 128]
            kb.tr(sl, src[:, j * 128:(j + 1) * 128], idt[:])
            kb.copy('act' if j % 2 else 'dve', dstT[:, j, :], sl)

    for t in range(NT):
        ts_ = slice(t * 128, (t + 1) * 128)
        kb.dma('sp', a0[:], ZA[0, ts_, :])
        kb.dma('act', a1[:], ZA[1, ts_, :])
        kb.dma('sp', a2_[:], GA[ts_, :])
        kb.tt('dve', a0[:], a0[:], a1[:], ALU.add)
        kb.tt('dve', a0[:], a0[:], a2_[:], ALU.mult)
        transposes(yaT, a0, 6, 0)
        kb.dma('sp', a0[:], OB[0, ts_, :])
        kb.dma('act', a1[:], OB[1, ts_, :])
        kb.dma('sp', a2_[:], OG[ts_, :])
        kb.tt('dve', a0[:], a0[:], a1[:], ALU.add)
        kb.tt('dve', a1[:], a0[:], a0[:], ALU.mult)
        kb.red(s8[:, 0:4], a1[:].rearrange("p (h v) -> p h v", h=4))
        _rms_rstd(kb, s8[:, 0:4], s8[:, 0:4], 192, 1e-6)
        kb.tt('dve', a0[:].rearrange("p (h v) -> p h v", h=4), a0[:].rearrange("p (h v) -> p h v", h=4),
              s8[:, 0:4].unsqueeze(2).broadcast_to([128, 4, 192]), ALU.mult)
        kb.tt('dve', a0[:], a0[:], ngb[:], ALU.mult)
        kb.actf(a2_[:], a2_[:], AF.Silu)
        kb.tt('dve', a0[:], a0[:], a2_[:], ALU.mult)
        transposes(ybT, a0, 6, 0)
        kb.dma('sp', yf[:, 0], YCF[0, :, ts_].rearrange("(j p) t -> p j t", p=128))
        kb.dma('act', yf[:, 1], YCF[1, :, ts_].rearrange("(j p) t -> p j t", p=128))
        kb.dma('sp', yf[:, 2], UF[:, ts_].rearrange("(j p) t -> p j t", p=128))
        kb.tt('dve', gt1[:], yf[:, 0], yf[:, 1], ALU.add)
        for j in range(4):
            kb.stt(gt1[:, j], yf[:, 2, j], dsk[:, j:j + 1], gt1[:, j], ALU.mult, ALU.add)
        kb.tt('dve', gt2[:], gt1[:], gt1[:], ALU.mult)
        kb.ts('dve', gt2[:], gt2[:], 0.044715, 1.0, ALU.mult, ALU.add)
        kb.tt('dve', gt2[:], gt2[:], gt1[:], ALU.mult)
        kb.actf(gt2[:], gt2[:], AF.Tanh, scale=0.7978845608028654)
        kb.ts('dve', gt2[:], gt2[:], 1.0, 0.5, ALU.add, ALU.mult)
        kb.tt('dve', gT[:], gt2[:], gt1[:], ALU.mult)
        for n in range(2):
            for j in range(4):
                kb.mm(ps[2 + n][:, :], gT[:, j, :], gluw[:, j, n * 512:(n + 1) * 512], start=(j == 0), stop=(j == 3))
        kb.actf(tq[:], ps[3][:, :], AF.Sigmoid)
        kb.tt('dve', yc[:], ps[2][:, :], tq[:], ALU.mult)
        transposes(ycT, yc, 4, 0)
        for n in range(4):
            ns = slice(n * 512, (n + 1) * 512)
            for br in range(3):
                kb.dma('sp' if br != 1 else 'act', bg[:, br, :], BG[ts_, br * D + n * 512:br * D + (n + 1) * 512])
            kb.actf(bg[:].rearrange("p a n -> p (a n)"), bg[:].rearrange("p a n -> p (a n)"), AF.Sigmoid)
            for k in range(6):
                kb.mm(ps[4][:, :], yaT[:, k, :], pa[:, k, ns], start=(k == 0), stop=(k == 5))
            for k in range(6):
                kb.mm(ps[5][:, :], ybT[:, k, :], pb[:, k, ns], start=(k == 0), stop=(k == 5))
            for k in range(4):
                kb.mm(ps[6][:, :], ycT[:, k, :], pc[:, k, ns], start=(k == 0), stop=(k == 3))
            kb.tt('dve', m[:, ns], ps[4][:, :], bg[:, 0, :], ALU.mult)
            kb.tt('dve', tq[:], ps[5][:, :], bg[:, 1, :], ALU.mult)
            kb.tt('dve', m[:, ns], m[:, ns], tq[:], ALU.add)
            kb.tt('dve', tq[:], ps[6][:, :], bg[:, 2, :], ALU.mult)
            kb.tt('dve', m[:, ns], m[:, ns], tq[:], ALU.add)
        kb.dma('sp', M[ts_, :], m[:])
    wo = wbig[:].rearrange("p (k n) -> p k n", k=16)
    kb.dma('pool', wo, WO.rearrange("(k p) n -> p k n", p=128))
    mT = kb.sbuf("mT", [128, 16, 128], BF16)
    xr = kb.sbuf("xr", [128, D])
    h2 = kb.sbuf("h2", [128, D])
    for t in range(NT):
        ts_ = slice(t * 128, (t + 1) * 128)
        if t <= 1:
            w = 0 if t == 0 else 1
            kb.dma('sp', modb[:].rearrange("p a n -> p (a n)"), MODB[w].rearrange("a p n -> p a n"))
            kb.stt(modb[:, 1], modb[:, 1], 1.0, g2b[:], ALU.add, ALU.mult)
        kb.dma('sp', m[:], M[ts_, :])
        kb.dma('act', xr[:], XIN[ts_, :])
        transposes(mT, m, 16, 0)
        for n in range(4):
            ns = slice(n * 512, (n + 1) * 512)
            p = ps[2 + n]
            for k in range(16):
                kb.mm(p[:, :], mT[:, k, :], wo[:, k, ns], start=(k == 0), stop=(k == 15))
            kb.tt('dve', h2[:, ns], p[:, :], modb[:, 0, ns], ALU.mult)
            kb.tt('dve', xr[:, ns], xr[:, ns], h2[:, ns], ALU.add)
        kb.dma('sp', XMID[ts_, :], xr[:])
        kb.actf(h2[:], xr[:], AF.Square)
        kb.red(s8[:, 4:5], h2[:])
        _rms_rstd(kb, s8[:, 4:5], s8[:, 4:5], D, 1e-6)
        kb.ts('dve', h2[:], xr[:], s8[:, 4:5], None, ALU.mult)
        kb.tt('dve', h2[:], h2[:], modb[:, 1], ALU.mult)
        kb.tt('dve', h2[:], h2[:], modb[:, 2], ALU.add)
        kb.dma('act', H2[ts_, :], h2[:])
    kb.finish('sp')
    kb.close()
    return nc


def build_L3b(NT=17, last=False):
    NTOK = NT * 128
    nc = bass.Bass("TRN2", target_bir_lowering=False)
    din = lambda n, s: nc.dram_tensor(n, s, F32, kind="ExternalInput").ap()
    H2T = din("H2T", [D, NTOK])
    XMID = din("XMID", [NTOK, D])
    GA2B = din("GA2B", [2, 128, D])
    RW = din("RW", [D, 16])
    RBB = din("RBB", [128, 16])
    W1 = din("W1", [16, D, 1024])
    W3 = din("W3", [16, D, 1024])
    W2 = din("W2", [16, 1024, D])
    FGB = din("FGB", [128, D])
    OUT = nc.dram_tensor("OUT", [NTOK, D], F32, kind="ExternalOutput").ap()
    GDBG = nc.dram_tensor("GDBG", [128, NT * 16], F32, kind="ExternalOutput").ap() if _GDBG else None
    kb = KB(nc)
    h2T = kb.sbuf("h2T", [128, 16, NTOK], BF16)
    H2v = H2T.rearrange("(k p) t -> p k t", p=128)
    for k in range(16):
        kb.dma('pool', h2T[:, k, :], H2v[:, k, :])
    rw = kb.sbuf("rw", [128, 16, 16])
    rbb = kb.sbuf("rbb", [128, 16])
    fgb = kb.sbuf("fgb", [128, D])
    ga2 = kb.sbuf("ga2", [128, D])
    kb.dma('sp', rw[:], RW.rearrange("(k p) e -> p k e", p=128))
    kb.dma('sp', rbb[:], RBB)
    kb.dma('sp', fgb[:], FGB)
    ps = [kb.psum("ps%d" % i, [128, 512]) for i in range(8)]
    gate = kb.sbuf("gate", [128, NT, 16])
    hf = [kb.sbuf("hf0", [128, 16, 128])] * 2
    R = kb.sbuf("R", [128, 8, 16])
    r4 = kb.sbuf("r4", [128, 8, 4])
    r1 = kb.sbuf("r1", [128, 8])
    g44 = lambda ap: ap.rearrange("p (g e) -> p g e", g=4)
    b44 = lambda ap: ap.unsqueeze(2).broadcast_to([128, 4, 4])
    for t in range(NT):
        h = hf[t % 2]
        kb.dma('sp' if t % 2 == 0 else 'act', h[:], H2v[:, :, t * 128:(t + 1) * 128])
        for k in range(16):
            kb.mm(ps[0][:, 0:16], h[:, k, :], rw[:, k, :], start=(k == 0), stop=(k == 15))
        sc, bi, mk, sel1, mk2, sel2 = [R[:, i] for i in range(6)]
        gs, t4_, ing, pen = [r4[:, i] for i in range(4)]
        kb.actf(sc, ps[0][:, 0:16], AF.Sigmoid)
        kb.tt('dve', bi, sc, rbb[:], ALU.add)
        bg_ = g44(bi)
        first = True
        for i in range(4):
            for j in range(i + 1, 4):
                if first:
                    kb.tt('dve', gs, bg_[:, :, i], bg_[:, :, j], ALU.add)
                    first = False
                else:
                    kb.tt('dve', t4_, bg_[:, :, i], bg_[:, :, j], ALU.add)
                    kb.tt('dve', gs, gs, t4_, ALU.max)
        kb.red(r1[:, 0:1], gs, ALU.max)
        kb.ts('dve', ing, gs, r1[:, 0:1], None, ALU.is_ge)
        kb.ts('dve', pen, ing, 1.0, 1e9, ALU.subtract, ALU.mult)
        kb.tt('dve', g44(mk), bg_, b44(ing), ALU.mult)
        kb.tt('dve', g44(mk), g44(mk), b44(pen), ALU.add)
        kb.red(r1[:, 1:2], mk, ALU.max)
        kb.ts('dve', sel1, mk, r1[:, 1:2], None, ALU.is_ge)
        kb.stt(mk2, sel1, -1e9, mk, ALU.mult, ALU.add)
        kb.red(r1[:, 2:3], mk2, ALU.max)
        kb.ts('dve', sel2, mk2, r1[:, 2:3], None, ALU.is_ge)
        kb.tt('dve', sel1, sel1, sel2, ALU.add)
        kb.tt('dve', sel1, sel1, sc, ALU.mult)
        kb.red(r1[:, 3:4], sel1)
        kb.op('dve', lambda g: g.reciprocal(r1[:, 4:5], r1[:, 3:4]), [r1[:, 4:5]], [r1[:, 3:4]])
        kb.ts('dve', gate[:, t, :], sel1, r1[:, 4:5], None, ALU.mult)
    if _GDBG:
        kb.dma('sp', GDBG, gate[:].rearrange("p t e -> p (t e)"))
    GS = _GS
    groups = [list(range(i, min(NT, i + GS))) for i in range(0, NT, GS)]
    acc = kb.sbuf("acc", [128, GS, D])
    wq = [dict(w1=kb.sbuf("w1q%d" % i, [128, 16, 256], BF16), w3=kb.sbuf("w3q%d" % i, [128, 16, 256], BF16),
               w2=kb.sbuf("w2q%d" % i, [128, 2, D], BF16)) for i in range(2)]
    s1 = kb.sbuf("s1", [128, 512])
    heT = [kb.sbuf("heT%d" % i, [128, 2, 512], BF16) for i in range(2)]
    xm = kb.sbuf("xm", [128, D])
    junk = hf[0][:].rearrange("p k t -> p (k t)")
    cw = 0
    cpo = 0
    che = 0
    for tiles in groups:
        if not tiles:
            continue
        chunks = [tiles[i:i + 4] for i in range(0, len(tiles), 4)]
        for e in range(16):
            for q in range(4):
                wb = wq[cw % 2]
                cw += 1
                fs = slice(q * 256, (q + 1) * 256)
                kb.dma('pool', wb['w1'][:], W1[e].rearrange("(k p) f -> p k f", p=128)[:, :, fs])
                kb.dma('pool', wb['w3'][:], W3[e].rearrange("(k p) f -> p k f", p=128)[:, :, fs])
                kb.dma('pool', wb['w2'][:], W2[e, fs, :].rearrange("(f p) n -> p f n", p=128))
                for ch in chunks:
                    tok0 = ch[0] * 128
                    ntok = len(ch) * 128
                    he = heT[che % 2]
                    che += 1
                    for f in range(2):
                        p1 = ps[1 + f]
                        p3 = ps[3 + f]
                        for k in range(16):
                            kb.mm(p1[:, 0:ntok], wb['w1'][:, k, f * 128:(f + 1) * 128], h2T[:, k, tok0:tok0 + ntok],
                                  start=(k == 0), stop=(k == 15))
                        for k in range(16):
                            kb.mm(p3[:, 0:ntok], wb['w3'][:, k, f * 128:(f + 1) * 128], h2T[:, k, tok0:tok0 + ntok],
                                  start=(k == 0), stop=(k == 15))
                        kb.actf(s1[:, 0:ntok], p1[:, 0:ntok], AF.Silu)
                        kb.tt('dve', he[:, f, 0:ntok], s1[:, 0:ntok], p3[:, 0:ntok], ALU.mult)
                    for ti, t in enumerate(ch):
                        for n in range(4):
                            po = ps[5 + cpo % 3]
                            cpo += 1
                            for f in range(2):
                                kb.mm(po[:, :], he[:, f, ti * 128:(ti + 1) * 128], wb['w2'][:, f, n * 512:(n + 1) * 512],
                                      start=(f == 0), stop=(f == 1))
                            a = acc[:, t - tiles[0], n * 512:(n + 1) * 512]
                            if e == 0 and q == 0:
                                kb.ts('dve', a, po[:, :], gate[:, t, e:e + 1], None, ALU.mult)
                            else:
                                kb.stt(a, po[:, :], gate[:, t, e:e + 1], a, ALU.mult, ALU.add)
        for t in tiles:
            ts_ = slice(t * 128, (t + 1) * 128)
            if t <= 1:
                kb.dma('sp', ga2[:], GA2B[0 if t == 0 else 1])
            a = acc[:, t - tiles[0], :]
            kb.dma('sp', xm[:], XMID[ts_, :])
            kb.tt('dve', a, a, ga2[:], ALU.mult)
            kb.tt('dve', xm[:], xm[:], a, ALU.add)
            if last:
                kb.actf(junk, xm[:], AF.Square)
                kb.red(r1[:, 5:6], junk)
                _rms_rstd(kb, r1[:, 5:6], r1[:, 5:6], D, 1e-6)
                kb.ts('dve', xm[:], xm[:], r1[:, 5:6], None, ALU.mult)
                kb.tt('dve', xm[:], xm[:], fgb[:], ALU.mult)
            kb.dma('act', OUT[ts_, :], xm[:])
    kb.finish('sp')
    kb.close()
    return nc


def _s5_lay(a):
    return np.ascontiguousarray(a.reshape(2, 8, 2, 64).transpose(2, 3, 0, 1).reshape(128, 16))


def kernel(x, c, ctx, c_ctx, mod_w, mod_b, norm1_g, norm2_g, w_in, conv_w,
           rk_w0, rk_w2, rk_a0, rk_a2, rk_g2, rk_kk, rk_ka, rk_rk, rk_gn_w, rk_gn_b,
           gla_a2, gla_ab, gla_norm_g,
           s5_lam_re, s5_lam_im, s5_log_dt, s5_b_re, s5_b_im, s5_c_re, s5_c_im, s5_d, s5_glu_w,
           proj_a, proj_b, proj_c, w_out, router_w, router_bias, exp_w1, exp_w3, exp_w2, final_g):
    f32 = np.float32
    A = lambda v: np.ascontiguousarray(np.asarray(v, dtype=f32))
    x = A(x).copy()
    ctx = A(ctx).copy()
    T = T_ALL
    ident = np.eye(128, dtype=f32)
    V = np.concatenate([A(c), A(c_ctx)[None]], 0)
    vt = A(V.reshape(5, 16, 128).transpose(2, 1, 0).reshape(128, 80))
    ims = []
    for core in range(8):
        l, q = core // 4, core % 4
        ims.append({"vt": vt, "W": A(mod_w[l][:, q * 3072:(q + 1) * 3072]),
                    "bias": A(np.repeat(mod_b[l][None, q * 3072:(q + 1) * 3072], 5, 0))})
    res = _run(build_L0(), ims)
    mod = [np.concatenate([res[l * 4 + q]["M"] for q in range(4)], 1) for l in range(2)]
    idx = [np.arange(T), np.concatenate([255 - np.arange(256), 4607 - np.arange(256, T)])]
    SEL = np.zeros((128, 64, 128), f32)
    for t in range(64):
        SEL[t, t, 0:64] = 1
        SEL[64 + t, t, 64:128] = 1
    SEL = SEL.reshape(128, -1)
    I2 = np.concatenate([np.eye(64), np.eye(64)], 0).astype(f32)
    TRI = np.concatenate([np.triu(np.ones((64, 64))), np.tril(np.ones((64, 64)), -1)], 1).astype(f32)
    nc1, nc2a, ncR, ncG, ncS, nc3a = build_L1(), build_L2a(), build_LRC(), build_LG(), build_LS(), build_L3a()
    CST = _lrc_consts()
    toks = [np.concatenate([np.arange(h * 128, (h + 1) * 128), 256 + np.arange(h * 2048, (h + 1) * 2048)]) for h in range(2)]
    for l in range(2):
        m = mod[l]
        mw = lambda b, j: m[b, j * D:(j + 1) * D]
        ims = []
        xins = []
        for core in range(8):
            b, h = core // 2, core % 2
            xin = A(np.concatenate([ctx[b, h * 128:(h + 1) * 128], x[b, h * 2048:(h + 1) * 2048]], 0))
            xins.append(xin)
            sc = np.concatenate([_colT(mw(4, 1)), _colT(mw(b, 1))], 1)
            sh = np.concatenate([_colT(mw(4, 0)), _colT(mw(b, 0))], 1)
            ims.append({"xin": xin, "W": A(w_in[l]), "sc": A(sc), "sh": A(sh), "g": _colT(A(norm1_g[l])), "ident": ident})
        res = _run(nc1, ims)
        P = np.empty((4, T, 11680), f32)
        for core in range(8):
            b, h = core // 2, core % 2
            P[b, toks[h]] = res[core]["P"]
        del res
        cw9 = A(conv_w[l]).reshape(9, 3840)
        ims = []
        for core in range(8):
            pj = np.empty((15 * 128, T), f32)
            cwm = np.empty((128, 15 * 9), f32)
            for i in range(15):
                b, tl = divmod(core * 15 + i, 30)
                pj[i * 128:(i + 1) * 128] = P[b, :, tl * 128:(tl + 1) * 128].T
                cwm[:, i * 9:(i + 1) * 9] = cw9[:, tl * 128:(tl + 1) * 128].T
            ims.append({"PJ": pj, "CW": cwm})
        res = _run(nc2a, ims)
        CV = np.empty((4, 3840, T), f32)
        for core in range(8):
            for i in range(15):
                b, tl = divmod(core * 15 + i, 30)
                CV[b, tl * 128:(tl + 1) * 128] = res[core]["CV"][i * 128:(i + 1) * 128]
        del res
        ims = []
        for core in range(8):
            b, s = core // 2, core % 2
            cs = slice(s * 384, (s + 1) * 384)
            r_fm, k_fm, v_fm = CV[b, 0:768][cs], CV[b, 768:1536][cs], CV[b, 1536:2304][cs]
            RK = np.stack([np.concatenate([r_fm[:, idx[d]].T, k_fm[:, idx[d]].T], 1) for d in range(2)])
            VS = np.stack([v_fm[:, idx[d]] for d in range(2)])
            ims.append({
                "RK": A(RK), "VTOK": A(VS.transpose(0, 2, 1)),
                "LW": A(np.stack([P[b, idx[d], 3840 + d * 64:3840 + (d + 1) * 64].T for d in range(2)])),
                "LA": A(np.stack([P[b, idx[d], 3968 + d * 64:3968 + (d + 1) * 64].T for d in range(2)])),
                "LG": A(P[b, :, 4096:4224].T),
                "W2": A(np.concatenate([rk_w2[l][:, :, cs], rk_w0[l][:, None, cs]], 1)),
                "A2": A(np.concatenate([rk_a2[l][:, :, cs], rk_a0[l][:, None, cs]], 1)),
                "G2": A(rk_g2[l][:, cs]),
                "BCS": A(np.concatenate([_bc(A(v[l])[cs]) for v in (rk_kk, rk_ka, rk_rk, rk_gn_w, rk_gn_b)], 1)),
                "CST": CST})
        res = _run(ncR, ims)
        ZA = np.empty((4, 2, T, 768), f32)
        GA = np.empty((4, T, 768), f32)
        for core in range(8):
            b, s = core // 2, core % 2
            for d in range(2):
                ZA[b, d, :, s * 384:(s + 1) * 384] = res[core]["Z"][d][idx[d]]
            GA[b, :, s * 384:(s + 1) * 384] = res[core]["G"]
        del res
        ims = []
        for core in range(8):
            b, s = core // 2, core % 2
            QKF = np.empty((4, 2, 96, T), f32)
            KT = np.empty((4, T, 96), f32)
            VT = np.empty((4, T, 192), f32)
            for u in range(4):
                d, hh = u // 2, u % 2
                h = 2 * s + hh
                q_fm = CV[b, 2304 + h * 96:2304 + (h + 1) * 96][:, idx[d]]
                k_fm = CV[b, 2688 + h * 96:2688 + (h + 1) * 96][:, idx[d]]
                v_fm = CV[b, 3072 + h * 192:3072 + (h + 1) * 192][:, idx[d]]
                QKF[u, 0], QKF[u, 1] = q_fm, k_fm
                KT[u] = k_fm.T
                VT[u] = v_fm.T
            ks = slice(s * 192, (s + 1) * 192)
            ims.append({"QKF": QKF, "KT": KT, "VT": VT,
                        "DA": A(np.stack([P[b, idx[d], 4224 + d * 16:4224 + (d + 1) * 16].T for d in range(2)])),
                        "A2": A(np.concatenate([gla_a2[l][:, :, ks], gla_ab[l][:, None, ks]], 1)), "TRI": TRI})
        res = _run(ncG, ims)
        OB = np.empty((4, 2, T, 768), f32)
        for core in range(8):
            b, s = core // 2, core % 2
            for d in range(2):
                OB[b, d, :, s * 384:(s + 1) * 384] = res[core]["O"][d][idx[d]]
        del res
        ims = []
        for core in range(8):
            b, s = core // 2, core % 2
            gsl = slice(16 * s, 16 * s + 16)
            UF = np.stack([P[b, idx[d], 5024 + s * 256:5024 + (s + 1) * 256].T for d in range(2)])
            LRI = np.concatenate([_s5_lay(A(s5_lam_re[l][:, gsl])), _s5_lay(A(s5_lam_im[l][:, gsl])),
                                  _s5_lay(A(np.broadcast_to(s5_log_dt[l][:, gsl, None], (2, 16, 64))))], 1)
            BT = np.zeros((32, 2, 8, 128), f32)
            CT = np.zeros((128, 2, 8, 32), f32)
            for j in range(8):
                for gg in range(2):
                    g = 16 * s + 2 * j + gg
                    BT[gg * 16:(gg + 1) * 16, 0, j, gg * 64:(gg + 1) * 64] = s5_b_re[l][g].T
                    BT[gg * 16:(gg + 1) * 16, 1, j, gg * 64:(gg + 1) * 64] = s5_b_im[l][g].T
                    CT[gg * 64:(gg + 1) * 64, 0, j, gg * 16:(gg + 1) * 16] = s5_c_re[l][g].T
                    CT[gg * 64:(gg + 1) * 64, 1, j, gg * 16:(gg + 1) * 16] = s5_c_im[l][g].T
            ims.append({"UF": A(UF), "LRI": A(LRI), "BT": BT.reshape(32, -1), "CT": CT.reshape(128, -1)})
        res = _run(ncS, ims)
        YCF = np.empty((4, 2, 512, T), f32)
        for core in range(8):
            b, s = core // 2, core % 2
            for d in range(2):
                YCF[b, d, s * 256:(s + 1) * 256] = res[core]["YC"][d][:, idx[d]]
        del res
        ims = []
        for core in range(8):
            b, h = core // 2, core % 2
            tk = toks[h]
            MODB = np.stack([np.stack([_bc(mw(w, 2)), _bc(mw(w, 4)), _bc(mw(w, 3))]) for w in (4, b)])
            ims.append({"XIN": xins[core], "ZA": A(ZA[b][:, tk]), "GA": A(GA[b][tk]), "OB": A(OB[b][:, tk]),
                        "OG": A(P[b, tk, 4256:5024]), "YCF": A(YCF[b][:, :, tk]), "UF": A(P[b, tk, 5024:5536].T),
                        "BG": A(P[b, tk, 5536:]), "MODB": A(MODB), "G2B": _bc(A(norm2_g[l])),
                        "NGB": _bc(np.tile(A(gla_norm_g[l]), 4)), "DSK": _colT(A(s5_d[l]), 4),
                        "PA": A(proj_a[l]), "PB": A(proj_b[l]), "PC": A(proj_c[l]), "WO": A(w_out[l]),
                        "GLU": A(s5_glu_w[l]), "ident": ident})
        res = _run(nc3a, ims)
        del P, CV, ZA, GA, OB, YCF
        last = (l == 1)
        ims2 = []
        for core in range(8):
            b, h = core // 2, core % 2
            ims2.append({"H2T": A(res[core]["H2"].T), "XMID": res[core]["XMID"],
                         "GA2B": A(np.stack([_bc(mw(4, 5)), _bc(mw(b, 5))])), "RW": A(router_w),
                         "RBB": _bc(A(router_bias)), "W1": A(exp_w1[l]), "W3": A(exp_w3[l]), "W2": A(exp_w2[l]),
                         "FGB": _bc(A(final_g))})
        del res
        res = _run(build_L3b(17, last), ims2)
        for core in range(8):
            b, h = core // 2, core % 2
            o = res[core]["OUT"]
            ctx[b, h * 128:(h + 1) * 128] = o[0:128]
            x[b, h * 2048:(h + 1) * 2048] = o[128:]
        del res
    return x


def _lrc_consts():
    f32 = np.float32
    C = np.zeros((128, 768), f32)
    s = np.arange(128)
    same = (s[:, None] // 64) == (s[None, :] // 64)
    C[:, 0:128] = same & (s[:, None] <= s[None, :])
    C[:, 128:256] = same & (s[:, None] > s[None, :])
    C[:, 256:384] = np.eye(128)
    r = np.arange(64)
    C[0:64, 384:448] = r[:, None] < r[None, :]
    C[0:64, 448:512] = r[:, None] > r[None, :]
    C[0:64, 512:576] = r[:, None] <= r[None, :]
    C[0:64, 576] = 1.0
    C[64:128, 577] = 1.0
    return C


def build_LRC(NCHUNK=NCH):
    T = NCHUNK * 64
    nc = bass.Bass("TRN2", target_bir_lowering=False)
    din = lambda n, s: nc.dram_tensor(n, s, F32, kind="ExternalInput").ap()
    RK = din("RK", [2, T, 768])
    VTOK = din("VTOK", [2, T, 384])
    LW = din("LW", [2, 64, T])
    LA = din("LA", [2, 64, T])
    LG = din("LG", [128, T])
    W2 = din("W2", [2, 65, 384])
    A2 = din("A2", [2, 65, 384])
    G2 = din("G2", [128, 384])
    BCS = din("BCS", [128, 5 * 384])
    CST = din("CST", [128, 768])
    Z = nc.dram_tensor("Z", [2, T, 384], F32, kind="ExternalOutput").ap()
    G = nc.dram_tensor("G", [T, 384], F32, kind="ExternalOutput").ap()
    TQ = nc.dram_tensor("TQ", [2, T, 4 * 384], F32, kind="Internal").ap()
    FQ = nc.dram_tensor("FQ", [2, 4, 6, 64, T], F32, kind="Internal").ap()
    kb = KB(nc)
    kb.track_dram("TQ")
    kb.track_dram("FQ")
    bcs = kb.sbuf("bcs", [128, 5, 6, 64])
    cst = kb.sbuf("cst", [128, 768])
    w2 = kb.sbuf("w2", [65, 2, 384])
    a2 = kb.sbuf("a2", [65, 2, 384])
    g2 = kb.sbuf("g2", [128, 384])
    kb.dma('sp', bcs[:].rearrange("p a h k -> p (a h k)"), BCS)
    kb.dma('sp', cst[:], CST)
    kb.dma('sp', g2[:], G2)
    for d in range(2):
        kb.dma('sp', w2[:, d, :], W2[d])
        kb.dma('sp', a2[:, d, :], A2[d])
    kkbc, kabc, rkbc, gnw, gnb = [bcs[:, i] for i in range(5)]
    tri2, aft2, idt = cst[:, 0:128], cst[:, 128:256], cst[:, 256:384]
    strict, lows, triinc = cst[0:64, 384:448], cst[0:64, 448:512], cst[0:64, 512:576]
    blk2 = cst[:, 576:578]
    ps = [kb.psum("ps%d" % i, [128, 512]) for i in range(8)]
    pl = kb.sbuf("pl", [64, 2, 6, NCHUNK])
    f3 = lambda ap: ap.rearrange("p h k -> p (h k)")
    lw = [kb.sbuf("lw%d" % i, [65, 128]) for i in range(2)]
    la = [kb.sbuf("la%d" % i, [65, 128]) for i in range(2)]
    lg = [kb.sbuf("lg%d" % i, [128, 128]) for i in range(2)]
    tq = [kb.sbuf("tq%d" % i, [128, 4, 6, 64]) for i in range(2)]
    kt = [kb.sbuf("kt%d" % i, [128, 6, 64]) for i in range(2)]
    fq = [kb.sbuf("fq%d" % i, [64, 4, 6, 128]) for i in range(2)]
    names = ["sg", "lwt", "at", "kk", "ka", "tA", "tB", "e1", "e2", "e3", "e4", "qa", "qb", "qk", "qr"]
    W_ = {n: kb.sbuf("w_" + n, [128, 6, 64]) for n in names}
    s6 = kb.sbuf("s6", [128, 8])
    gt = [kb.sbuf("gt%d" % i, [128, 384]) for i in range(2)]
    for i in range(2):
        kb.memset('dve', lw[i][:], 1.0)
        kb.memset('dve', la[i][:], 1.0)
    it = 0
    for d in range(2):
        for t in range(T // 128):
            b = it % 2
            it += 1
            ts_ = slice(t * 128, (t + 1) * 128)
            q = tq[b]
            k_ = kt[b]
            kb.dma('sp', f3(q[:, 2]), RK[d, ts_, 0:384])
            kb.dma('sp', f3(k_[:]), RK[d, ts_, 384:768])
            kb.dma('act', lw[b][0:64, :], LW[d, :, ts_])
            kb.dma('act', la[b][0:64, :], LA[d, :, ts_])
            kb.actf(lw[b][0:64, :], lw[b][0:64, :], AF.Tanh)
            kb.mm(ps[0][:, 0:384], lw[b][:, :], w2[:, d, :])
            kb.mm(ps[1][:, 0:384], la[b][:, :], a2[:, d, :])
            kb.actf(f3(W_["sg"][:]), ps[0][:, 0:384], AF.Sigmoid)
            kb.ts('dve', W_["lwt"][:], W_["sg"][:], -A_DECAY_SCALE, None, ALU.mult)
            kb.actf(f3(W_["at"][:]), ps[1][:, 0:384], AF.Sigmoid)
            kb.tt('dve', W_["tA"][:], k_[:], kkbc, ALU.mult)
            kb.tt('dve', W_["tB"][:], W_["tA"][:], W_["tA"][:], ALU.mult)
            kb.red(s6[:, 0:6], W_["tB"][:])
            kb.ts('dve', s6[:, 0:6], s6[:, 0:6], 1e-12, None, ALU.add)
            kb.actf(s6[:, 0:6], s6[:, 0:6], AF.Sqrt)
            kb.op('dve', lambda g: g.reciprocal(s6[:, 0:6], s6[:, 0:6]), [s6[:, 0:6]], [s6[:, 0:6]])
            kb.tt('dve', W_["kk"][:], W_["tA"][:], s6[:, 0:6].unsqueeze(2).broadcast_to([128, 6, 64]), ALU.mult)
            kb.tt('dve', W_["ka"][:], W_["kk"][:], W_["at"][:], ALU.mult)
            kb.stt(W_["tB"][:], W_["at"][:], -1.0, kabc, ALU.add, ALU.mult)
            kb.stt(q[:, 3], W_["tB"][:], 1.0, k_[:], ALU.add, ALU.mult)
            kb.mm(ps[2][:, 0:384], tri2, f3(W_["lwt"][:]))
            kb.mm(ps[3][:, 0:384], aft2, f3(W_["lwt"][:]))
            kb.actf(f3(W_["e3"][:]), ps[2][:, 0:384], AF.Exp)
            kb.actf(f3(W_["e2"][:]), ps[2][:, 0:384], AF.Exp, scale=-1.0)
            kb.actf(f3(W_["e4"][:]), ps[3][:, 0:384], AF.Exp)
            kb.tt('dve', f3(W_["tA"][:]), ps[2][:, 0:384], f3(W_["lwt"][:]), ALU.subtract)
            kb.actf(W_["e1"][:], W_["tA"][:], AF.Exp)
            kb.stt(W_["qa"][:], W_["kk"][:], -1.0, W_["e1"][:], ALU.mult, ALU.mult)
            kb.tt('dve', W_["qb"][:], W_["ka"][:], W_["e2"][:], ALU.mult)
            kb.tt('dve', W_["qk"][:], q[:, 3], W_["e2"][:], ALU.mult)
            kb.tt('dve', W_["qr"][:], q[:, 2], W_["e3"][:], ALU.mult)
            kb.tt('dve', q[:, 0], W_["ka"][:], W_["e4"][:], ALU.mult)
            kb.tt('dve', q[:, 1], q[:, 3], W_["e4"][:], ALU.mult)
            kb.dma('sp', TQ[d, ts_, :], q[:].rearrange("p a h k -> p (a h k)"))
            fqt = fq[b]
            for qi, nm in enumerate(["qa", "qb", "qk", "qr"]):
                pA, pB = ps[4 + 2 * (qi % 2)], ps[5 + 2 * (qi % 2)]
                for h in range(6):
                    dst = (pA if h < 4 else pB)[0:64, (h % 4) * 128:(h % 4 + 1) * 128]
                    kb.tr(dst, W_[nm][:, h, :], idt)
                kb.copy('act', fqt[:, qi, 0:4, :], pA[0:64, 0:512].rearrange("p (h t) -> p h t", h=4))
                kb.copy('act', fqt[:, qi, 4:6, :], pB[0:64, 0:256].rearrange("p (h t) -> p h t", h=2))
                kb.dma('act' if qi % 2 else 'sp', FQ[d, qi].rearrange("h k t -> k h t")[:, :, ts_], fqt[:, qi])
            for h in range(6):
                kb.mm(ps[1][0:64, 400 + 2 * h:402 + 2 * h], W_["lwt"][:, h, :], blk2)
            kb.actf(pl[:, d, :, 2 * t:2 * t + 2], ps[1][0:64, 400:412].rearrange("p (h c) -> p h c", h=6), AF.Exp)
            if d == 0:
                kb.dma('act', lg[b][:], LG[:, ts_])
                kb.actf(lg[b][:], lg[b][:], AF.Sigmoid)
                kb.mm(ps[0][:, 0:384], lg[b][:], g2[:])
                kb.copy('act', gt[b][:], ps[0][:, 0:384])
                kb.dma('sp', G[ts_, :], gt[b][:])
    units = [(d, h) for d in range(2) for h in range(6)]
    ST = [kb.sbuf("ST%d" % u, [64, 64]) for u in range(12)]
    scr = [kb.sbuf("scr%d" % u, [64, 8, 64]) for u in range(12)]
    for u in range(12):
        kb.memset('dve', ST[u][:], 0.0)
    fqc = [kb.sbuf("fqc%d" % i, [64, 2, 4, 6, 64]) for i in range(2)]
    tqc = [kb.sbuf("tqc%d" % i, [64, 2, 4, 6, 64]) for i in range(2)]
    vtc = [kb.sbuf("vtc%d" % i, [64, 2, 6, 64]) for i in range(2)]
    yt = kb.sbuf("yt", [64, 6, 64])
    cen = kb.sbuf("cen", [64, 6, 64])
    zt = [kb.sbuf("zt%d" % i, [64, 6, 64]) for i in range(2)]
    st = kb.sbuf("st", [64, 24])
    bc6 = lambda ap: ap.unsqueeze(2).broadcast_to([64, 6, 64])
    pcount = [0]

    def pb():
        p = ps[pcount[0] % 6]
        pcount[0] += 1
        return p[0:64, 0:64]

    zi = 0
    for c in range(NCHUNK):
        b = c % 2
        cs = slice(c * 64, (c + 1) * 64)
        for d in range(2):
            e1_, e2_ = ('sp', 'act') if d == 0 else ('act', 'sp')
            for qi in range(4):
                kb.dma(e1_ if qi % 2 == 0 else e2_, fqc[b][:, d, qi], FQ[d, qi].rearrange("h k t -> k h t")[:, :, cs])
            kb.dma(e2_, tqc[b][:, d].rearrange("p a h k -> p (a h k)"), TQ[d, cs, :])
            kb.dma(e1_, vtc[b][:, d].rearrange("p h k -> p (h k)"), VTOK[d, cs, :])
        F = lambda u, qi: fqc[b][:, units[u][0], qi, units[u][1], :]
        TQs = lambda u, a: tqc[b][:, units[u][0], a, units[u][1], :]
        Vh = lambda u: vtc[b][:, units[u][0], units[u][1], :]
        for u in range(12):
            p = pb()
            kb.mm(p, F(u, 1), F(u, 0))
            kb.tt('dve', scr[u][:, 0], p, strict, ALU.mult)
        for u in range(12):
            p = pb()
            kb.mm(p, F(u, 0), F(u, 1))
            kb.tt('dve', scr[u][:, 2], p, lows, ALU.mult)
        for u in range(12):
            p = pb()
            kb.mm(p, F(u, 2), F(u, 0))
            kb.tt('dve', scr[u][:, 4], p, strict, ALU.mult)
        for u in range(12):
            p = pb()
            kb.mm(p, F(u, 0), ST[u][:], start=True, stop=False)
            kb.mm(p, scr[u][:, 4], Vh(u), start=False, stop=True)
            kb.copy('act', scr[u][:, 5], p)
        for i in range(6):
            ni, nti = i % 2, 2 + i % 2
            nn, ntn = (i + 1) % 2, 2 + (i + 1) % 2
            for u in range(12):
                p = pb()
                kb.mm(p, scr[u][:, ni], scr[u][:, 5])
                kb.tt('dve', scr[u][:, 5], p, scr[u][:, 5], ALU.add)
            if i < 5:
                for u in range(12):
                    p = pb()
                    kb.mm(p, scr[u][:, nti], scr[u][:, ni])
                    p2 = pb()
                    kb.mm(p2, scr[u][:, ni], scr[u][:, nti])
                    kb.copy('act', scr[u][:, nn], p)
                    kb.copy('act', scr[u][:, ntn], p2)
        for u in range(12):
            p = pb()
            kb.mm(p, F(u, 1), F(u, 3))
            kb.tt('dve', scr[u][:, 6], p, triinc, ALU.mult)
            p = pb()
            kb.mm(p, F(u, 2), F(u, 3))
            kb.tt('dve', scr[u][:, 7], p, triinc, ALU.mult)
        for u in range(12):
            d, h = units[u]
            py = ps[6 + d][0:64, h * 64:(h + 1) * 64]
            kb.mm(py, F(u, 3), ST[u][:], start=True, stop=False)
            kb.mm(py, scr[u][:, 6], scr[u][:, 5], start=False, stop=False)
            kb.mm(py, scr[u][:, 7], Vh(u), start=False, stop=True)
        for u in range(12):
            d, h = units[u]
            p = pb()
            kb.mm(p, TQs(u, 0), scr[u][:, 5], start=True, stop=False)
            kb.mm(p, TQs(u, 1), Vh(u), start=False, stop=True)
            kb.stt(ST[u][:], ST[u][:], pl[:, d, h, c:c + 1], p, ALU.mult, ALU.add)
        for d in range(2):
            kb.copy('act', f3(yt[:]), ps[6 + d][0:64, 0:384])
            kb.red(st[:, 0:6], yt[:])
            kb.ts('dve', st[:, 0:6], st[:, 0:6], -1.0 / 64, None, ALU.mult)
            kb.tt('dve', cen[:], yt[:], bc6(st[:, 0:6]), ALU.add)
            kb.tt('dve', yt[:], cen[:], cen[:], ALU.mult)
            kb.red(st[:, 6:12], yt[:])
            _rms_rstd(kb, st[:, 6:12], st[:, 6:12], 64, 64e-5)
            kb.tt('dve', cen[:], cen[:], bc6(st[:, 6:12]), ALU.mult)
            kb.tt('dve', cen[:], cen[:], gnw[0:64], ALU.mult)
            kb.tt('dve', cen[:], cen[:], gnb[0:64], ALU.add)
            kb.tt('dve', yt[:], tqc[b][:, d, 2], tqc[b][:, d, 3], ALU.mult)
            kb.tt('dve', yt[:], yt[:], rkbc[0:64], ALU.mult)
            kb.red(st[:, 12:18], yt[:])
            z = zt[zi % 2]
            zi += 1
            kb.tt('dve', z[:], vtc[b][:, d], bc6(st[:, 12:18]), ALU.mult)
            kb.tt('dve', z[:], z[:], cen[:], ALU.add)
            kb.dma('sp' if d == 0 else 'act', Z[d, cs, :], f3(z[:]))
    kb.finish('sp')
    kb.close()
    return nc
```
